# Optimizing a Trainium2 kernel written in Bass

```python
import math
import jax
import jax.numpy as jnp
from jax import lax
import numpy as np

D_MODEL = 1024
BATCH = 2
SEQ = 16384
DEPTH = 4

N_MIXERS = 3
NORM_EPS = 1e-6

GM_CHUNK = 128
GM_WIDTH = D_MODEL
GM_GROUPS = 8
GM_GROUP_DIM = GM_WIDTH // GM_GROUPS

DN_QK_HEADS = 4
DN_V_HEADS = 8
DN_HEAD_DIM = D_MODEL // DN_V_HEADS
DN_CONV = 4
DN_CHUNK = 64
DN_KEY_WIDTH = DN_QK_HEADS * DN_HEAD_DIM
DN_VAL_WIDTH = DN_V_HEADS * DN_HEAD_DIM
DN_CONV_WIDTH = 2 * DN_KEY_WIDTH + DN_VAL_WIDTH
DN_PROJ_WIDTH = DN_CONV_WIDTH + DN_VAL_WIDTH + 2 * DN_V_HEADS

SWA_HEAD_DIM = 64
SWA_Q_HEADS = D_MODEL // SWA_HEAD_DIM
SWA_KV_HEADS = 4
SWA_GROUP = SWA_Q_HEADS // SWA_KV_HEADS
SWA_WINDOW = 128
SWA_BLOCK = 128
SWA_PROJ_WIDTH = (SWA_Q_HEADS + 2 * SWA_KV_HEADS) * SWA_HEAD_DIM

MOE_GROUPS = 4
MOE_EXPERTS_PER_GROUP = 8
MOE_EXPERTS = MOE_GROUPS * MOE_EXPERTS_PER_GROUP
MOE_TOP_K = 2
MOE_FF = D_MODEL // 2
MOE_BLOCK = 128

kernel_name = 'hybrid_gmlp_deltanet_swa_hmoe'


def rms_norm(x, gain):
    xf = x.astype(jnp.float32)
    y = xf * lax.rsqrt(jnp.mean(xf * xf, axis=-1, keepdims=True) + NORM_EPS)
    return (y * gain.astype(jnp.float32)).astype(x.dtype)


def l2_norm(x):
    xf = x.astype(jnp.float32)
    return xf * lax.rsqrt(jnp.sum(xf * xf, axis=-1, keepdims=True) + NORM_EPS)


def causal_depthwise_conv(x, w):
    s = x.shape[1]
    taps = w.shape[0]
    xp = jnp.pad(x, ((0, 0), (taps - 1, 0), (0, 0)))
    return sum(xp[:, j:j + s] * w[j] for j in range(taps))


def gmlp_mixer(h, w_in, v_norm, w_s, b_s, w_out):
    b, s, _ = h.shape
    z = jax.nn.gelu(h @ w_in)
    u, v = jnp.split(z, 2, axis=-1)
    v = rms_norm(v, v_norm)
    v = v.reshape(b, s // GM_CHUNK, GM_CHUNK, GM_GROUPS, GM_GROUP_DIM)
    causal = jnp.tril(jnp.ones((GM_CHUNK, GM_CHUNK), dtype=bool))
    w_causal = jnp.where(causal, w_s, 0).astype(v.dtype)
    sv = jnp.einsum('gts,bcsgd->bctgd', w_causal, v) + b_s.T[:, :, None]
    return (u * sv.reshape(b, s, GM_WIDTH)) @ w_out


def _to_chunks(t):
    b, s, h, d = t.shape
    return t.reshape(b, s // DN_CHUNK, DN_CHUNK, h, d).transpose(0, 3, 1, 2, 4)


def chunk_gated_delta_rule(q, k, v, g, beta):
    b, s, h, dk = q.shape
    dv = v.shape[-1]
    n = s // DN_CHUNK
    q = _to_chunks(q) * dk ** -0.5
    k = _to_chunks(k)
    v = _to_chunks(v)
    beta = beta.reshape(b, n, DN_CHUNK, h).transpose(0, 3, 1, 2)
    gc = jnp.cumsum(g.reshape(b, n, DN_CHUNK, h).transpose(0, 3, 1, 2), axis=-1)
    lower = jnp.tril(jnp.ones((DN_CHUNK, DN_CHUNK), dtype=bool))
    strict = jnp.tril(jnp.ones((DN_CHUNK, DN_CHUNK), dtype=bool), k=-1)
    diff = gc[..., :, None] - gc[..., None, :]
    decay = jnp.where(lower, jnp.exp(jnp.where(lower, diff, 0.0)), 0.0)
    k_beta = k * beta[..., None]
    v_beta = v * beta[..., None]
    kk = jnp.einsum('bhncd,bhnsd->bhncs', k_beta, k) * decay
    tri = jnp.where(strict, kk, 0.0) + jnp.eye(DN_CHUNK, dtype=jnp.float32)
    u = lax.linalg.triangular_solve(tri, v_beta, left_side=True, lower=True, unit_diagonal=True)
    w = lax.linalg.triangular_solve(tri, k_beta * jnp.exp(gc)[..., None],
                                    left_side=True, lower=True, unit_diagonal=True)
    qk = jnp.einsum('bhncd,bhnsd->bhncs', q, k) * decay
    q_dec = q * jnp.exp(gc)[..., None]
    k_dec = k * jnp.exp(gc[..., -1:] - gc)[..., None]
    g_last = jnp.exp(gc[..., -1])

    def step(state, inp):
        u_n, w_n, qk_n, qd_n, kd_n, gl_n = inp
        v_new = u_n - jnp.einsum('bhcd,bhde->bhce', w_n, state)
        o_n = jnp.einsum('bhcd,bhde->bhce', qd_n, state) + jnp.einsum('bhcs,bhse->bhce', qk_n, v_new)
        state = state * gl_n[..., None, None] + jnp.einsum('bhcd,bhce->bhde', kd_n, v_new)
        return state, o_n

    xs = tuple(jnp.moveaxis(t, 2, 0) for t in (u, w, qk, q_dec, k_dec, g_last))
    state0 = jnp.zeros((b, h, dk, dv), jnp.float32)
    _, o = lax.scan(step, state0, xs)
    return o.transpose(1, 0, 3, 2, 4).reshape(b, s, h, dv)


def deltanet_mixer(h, w_in, conv_w, a_log, dt_bias, o_norm, w_out):
    b, s, _ = h.shape
    proj = h @ w_in
    split_at = [DN_CONV_WIDTH, DN_CONV_WIDTH + DN_VAL_WIDTH, DN_CONV_WIDTH + DN_VAL_WIDTH + DN_V_HEADS]
    qkv, z, beta_in, alpha_in = jnp.split(proj, split_at, axis=-1)
    qkv = jax.nn.silu(causal_depthwise_conv(qkv, conv_w))
    q, k, v = jnp.split(qkv, [DN_KEY_WIDTH, 2 * DN_KEY_WIDTH], axis=-1)
    rep = DN_V_HEADS // DN_QK_HEADS
    q = jnp.repeat(l2_norm(q.reshape(b, s, DN_QK_HEADS, DN_HEAD_DIM)), rep, axis=2)
    k = jnp.repeat(l2_norm(k.reshape(b, s, DN_QK_HEADS, DN_HEAD_DIM)), rep, axis=2)
    v = v.reshape(b, s, DN_V_HEADS, DN_HEAD_DIM).astype(jnp.float32)
    beta = jax.nn.sigmoid(beta_in.astype(jnp.float32))
    g = -jnp.exp(a_log.astype(jnp.float32)) * jax.nn.softplus(
        alpha_in.astype(jnp.float32) + dt_bias.astype(jnp.float32))
    o = chunk_gated_delta_rule(q, k, v, g, beta)
    o = rms_norm(o, o_norm) * jax.nn.silu(z.reshape(b, s, DN_V_HEADS, DN_HEAD_DIM).astype(jnp.float32))
    return o.reshape(b, s, DN_VAL_WIDTH).astype(h.dtype) @ w_out


def swa_mixer(h, w_in, q_norm, k_norm, sinks, w_out):
    b, s, _ = h.shape
    nb = s // SWA_BLOCK
    hd = SWA_HEAD_DIM
    proj = h @ w_in
    q, k, v = jnp.split(proj, [SWA_Q_HEADS * hd, (SWA_Q_HEADS + SWA_KV_HEADS) * hd], axis=-1)
    q = rms_norm(q.reshape(b, s, SWA_Q_HEADS, hd), q_norm).astype(jnp.float32) * hd ** -0.5
    k = rms_norm(k.reshape(b, s, SWA_KV_HEADS, hd), k_norm).astype(jnp.float32)
    v = v.reshape(b, s, SWA_KV_HEADS, hd).astype(jnp.float32)
    q = q.reshape(b, nb, SWA_BLOCK, SWA_KV_HEADS, SWA_GROUP, hd)
    k = k.reshape(b, nb, SWA_BLOCK, SWA_KV_HEADS, hd)
    v = v.reshape(b, nb, SWA_BLOCK, SWA_KV_HEADS, hd)

    def with_prev(t):
        prev = jnp.concatenate([jnp.zeros_like(t[:, :1]), t[:, :-1]], axis=1)
        return jnp.concatenate([prev, t], axis=2)

    kk, vv = with_prev(k), with_prev(v)
    scores = jnp.einsum('bnqhgd,bnkhd->bnhgqk', q, kk)
    qi = jnp.arange(SWA_BLOCK)[:, None]
    kj = jnp.arange(2 * SWA_BLOCK)[None, :]
    offset = SWA_BLOCK + qi - kj
    band = (offset >= 0) & (offset < SWA_WINDOW)
    has_key = (jnp.arange(nb) > 0)[:, None, None] | (kj >= SWA_BLOCK)[None]
    mask = band[None] & has_key
    scores = jnp.where(mask[None, :, None, None], scores, -jnp.inf)
    sink = sinks.astype(jnp.float32).reshape(SWA_KV_HEADS, SWA_GROUP)[None, None, :, :, None, None]
    m = jnp.maximum(scores.max(axis=-1, keepdims=True), sink)
    p = jnp.exp(scores - m)
    denom = p.sum(axis=-1, keepdims=True) + jnp.exp(sink - m)
    out = jnp.einsum('bnhgqk,bnkhd->bnqhgd', p / denom, vv)
    return out.reshape(b, s, SWA_Q_HEADS * hd).astype(h.dtype) @ w_out


def hierarchical_moe(h, router_w, router_b, w_in, w_out):
    b, s, d = h.shape
    t = b * s
    xt = h.reshape(t, d)
    logits = (xt @ router_w + router_b).astype(jnp.float32)
    p_group = jax.nn.softmax(logits[:, :MOE_GROUPS], axis=-1)
    pg_top, g_sel = lax.top_k(p_group, 1)
    exp_logits = logits[:, MOE_GROUPS:].reshape(t, MOE_GROUPS, MOE_EXPERTS_PER_GROUP)
    sel_logits = jnp.take_along_axis(exp_logits, g_sel[:, :, None], axis=1)[:, 0]
    p_exp = jax.nn.softmax(sel_logits, axis=-1)
    pe_top, e_sel = lax.top_k(p_exp, MOE_TOP_K)
    gates = pg_top * pe_top / pe_top.sum(axis=-1, keepdims=True)
    expert_id = g_sel * MOE_EXPERTS_PER_GROUP + e_sel

    n_assign = t * MOE_TOP_K
    flat_e = expert_id.reshape(n_assign)
    flat_tok = jnp.repeat(jnp.arange(t, dtype=jnp.int32), MOE_TOP_K)
    flat_gate = gates.reshape(n_assign)
    order = jnp.argsort(flat_e)
    e_sorted = flat_e[order]
    tok_sorted = flat_tok[order]
    gate_sorted = flat_gate[order]
    counts = jnp.bincount(flat_e, length=MOE_EXPERTS)
    padded = (counts + MOE_BLOCK - 1) // MOE_BLOCK * MOE_BLOCK
    pad_end = jnp.cumsum(padded)
    pad_start = pad_end - padded
    seg_start = jnp.cumsum(counts) - counts
    dest = pad_start[e_sorted] + jnp.arange(n_assign) - seg_start[e_sorted]
    n_blocks = n_assign // MOE_BLOCK + MOE_EXPERTS
    slot_tok = jnp.full((n_blocks * MOE_BLOCK,), t, dtype=jnp.int32).at[dest].set(tok_sorted)
    x_pad = jnp.concatenate([xt, jnp.zeros((1, d), xt.dtype)], axis=0)
    x_blocks = x_pad[slot_tok].reshape(n_blocks, MOE_BLOCK, d)
    block_e = jnp.minimum(jnp.searchsorted(pad_end, jnp.arange(n_blocks) * MOE_BLOCK, side='right'),
                          MOE_EXPERTS - 1)

    def expert_block(args):
        xb, e = args
        a, gl = jnp.split(xb @ w_in[e], 2, axis=-1)
        return (jax.nn.silu(a) * gl) @ w_out[e]

    y_blocks = lax.map(expert_block, (x_blocks, block_e))
    y_slots = y_blocks.reshape(n_blocks * MOE_BLOCK, d)
    contrib = y_slots[dest] * gate_sorted[:, None].astype(h.dtype)
    out = jax.ops.segment_sum(contrib, tok_sorted, num_segments=t)
    return out.reshape(b, s, d)


def setup_inputs(seed: int = 0) -> dict:
    key = jax.random.key(seed)
    keys = iter(jax.random.split(key, 128))

    def normal(shape, scale):
        return jax.random.normal(next(keys), shape, jnp.float32) * scale

    def gain(n):
        return 1.0 + 0.05 * normal((n,), 1.0)

    p = {}
    p['x'] = normal((BATCH, SEQ, D_MODEL), 1.0)
    p['c'] = normal((BATCH, D_MODEL), 1.0)
    for i in range(DEPTH):
        pre = 'l%d_' % i
        p[pre + 'norm_mix'] = gain(D_MODEL)
        p[pre + 'norm_ffn'] = gain(D_MODEL)
        p[pre + 'ada_w'] = normal((D_MODEL, 6 * D_MODEL), 0.5 * D_MODEL ** -0.5)
        p[pre + 'ada_b'] = normal((6 * D_MODEL,), 0.02)
        kind = i % N_MIXERS
        if kind == 0:
            p[pre + 'gm_w_in'] = normal((D_MODEL, 2 * GM_WIDTH), D_MODEL ** -0.5)
            p[pre + 'gm_v_norm'] = gain(GM_WIDTH)
            p[pre + 'gm_w_s'] = normal((GM_GROUPS, GM_CHUNK, GM_CHUNK), GM_CHUNK ** -0.5)
            p[pre + 'gm_b_s'] = 1.0 + normal((GM_GROUPS, GM_CHUNK), 0.1)
            p[pre + 'gm_w_out'] = normal((GM_WIDTH, D_MODEL), GM_WIDTH ** -0.5)
        elif kind == 1:
            p[pre + 'dn_w_in'] = normal((D_MODEL, DN_PROJ_WIDTH), D_MODEL ** -0.5)
            p[pre + 'dn_conv_w'] = normal((DN_CONV, DN_CONV_WIDTH), DN_CONV ** -0.5)
            p[pre + 'dn_a_log'] = jnp.log(jax.random.uniform(next(keys), (DN_V_HEADS,), jnp.float32, 1.0, 16.0))
            dt = jnp.exp(jax.random.uniform(next(keys), (DN_V_HEADS,), jnp.float32,
                                            math.log(1e-3), math.log(1e-1)))
            p[pre + 'dn_dt_bias'] = dt + jnp.log(-jnp.expm1(-dt))
            p[pre + 'dn_o_norm'] = gain(DN_HEAD_DIM)
            p[pre + 'dn_w_out'] = normal((DN_VAL_WIDTH, D_MODEL), DN_VAL_WIDTH ** -0.5)
        else:
            p[pre + 'swa_w_in'] = normal((D_MODEL, SWA_PROJ_WIDTH), D_MODEL ** -0.5)
            p[pre + 'swa_q_norm'] = gain(SWA_HEAD_DIM)
            p[pre + 'swa_k_norm'] = gain(SWA_HEAD_DIM)
            p[pre + 'swa_sinks'] = normal((SWA_Q_HEADS,), 1.0)
            p[pre + 'swa_w_out'] = normal((SWA_Q_HEADS * SWA_HEAD_DIM, D_MODEL), (SWA_Q_HEADS * SWA_HEAD_DIM) ** -0.5)
        p[pre + 'router_w'] = normal((D_MODEL, MOE_GROUPS + MOE_EXPERTS), D_MODEL ** -0.5)
        p[pre + 'router_b'] = normal((MOE_GROUPS + MOE_EXPERTS,), 0.01)
        p[pre + 'expert_w_in'] = normal((MOE_EXPERTS, D_MODEL, 2 * MOE_FF), D_MODEL ** -0.5)
        p[pre + 'expert_w_out'] = normal((MOE_EXPERTS, MOE_FF, D_MODEL), MOE_FF ** -0.5)
    return p


def reference(x, c,
              l0_norm_mix, l0_norm_ffn, l0_ada_w, l0_ada_b,
              l0_gm_w_in, l0_gm_v_norm, l0_gm_w_s, l0_gm_b_s, l0_gm_w_out,
              l0_router_w, l0_router_b, l0_expert_w_in, l0_expert_w_out,
              l1_norm_mix, l1_norm_ffn, l1_ada_w, l1_ada_b,
              l1_dn_w_in, l1_dn_conv_w, l1_dn_a_log, l1_dn_dt_bias, l1_dn_o_norm, l1_dn_w_out,
              l1_router_w, l1_router_b, l1_expert_w_in, l1_expert_w_out,
              l2_norm_mix, l2_norm_ffn, l2_ada_w, l2_ada_b,
              l2_swa_w_in, l2_swa_q_norm, l2_swa_k_norm, l2_swa_sinks, l2_swa_w_out,
              l2_router_w, l2_router_b, l2_expert_w_in, l2_expert_w_out,
              l3_norm_mix, l3_norm_ffn, l3_ada_w, l3_ada_b,
              l3_gm_w_in, l3_gm_v_norm, l3_gm_w_s, l3_gm_b_s, l3_gm_w_out,
              l3_router_w, l3_router_b, l3_expert_w_in, l3_expert_w_out):
    layers = [
        ((l0_norm_mix, l0_norm_ffn, l0_ada_w, l0_ada_b),
         (l0_gm_w_in, l0_gm_v_norm, l0_gm_w_s, l0_gm_b_s, l0_gm_w_out),
         (l0_router_w, l0_router_b, l0_expert_w_in, l0_expert_w_out)),
        ((l1_norm_mix, l1_norm_ffn, l1_ada_w, l1_ada_b),
         (l1_dn_w_in, l1_dn_conv_w, l1_dn_a_log, l1_dn_dt_bias, l1_dn_o_norm, l1_dn_w_out),
         (l1_router_w, l1_router_b, l1_expert_w_in, l1_expert_w_out)),
        ((l2_norm_mix, l2_norm_ffn, l2_ada_w, l2_ada_b),
         (l2_swa_w_in, l2_swa_q_norm, l2_swa_k_norm, l2_swa_sinks, l2_swa_w_out),
         (l2_router_w, l2_router_b, l2_expert_w_in, l2_expert_w_out)),
        ((l3_norm_mix, l3_norm_ffn, l3_ada_w, l3_ada_b),
         (l3_gm_w_in, l3_gm_v_norm, l3_gm_w_s, l3_gm_b_s, l3_gm_w_out),
         (l3_router_w, l3_router_b, l3_expert_w_in, l3_expert_w_out)),
    ]
    mixers = (gmlp_mixer, deltanet_mixer, swa_mixer)
    c_act = jax.nn.silu(c)
    for i in range(DEPTH):
        (norm_mix, norm_ffn, ada_w, ada_b), mixer_params, moe_params = layers[i]
        mod = (c_act @ ada_w + ada_b)[:, None, :]
        shift_m, scale_m, gate_m, shift_f, scale_f, gate_f = jnp.split(mod, 6, axis=-1)
        h = rms_norm(x, norm_mix) * (1 + scale_m) + shift_m
        x = x + gate_m * mixers[i % N_MIXERS](h, *mixer_params)
        h = rms_norm(x, norm_ffn) * (1 + scale_f) + shift_f
        x = x + gate_f * hierarchical_moe(h, *moe_params)
    return x
```

```python
import contextlib
import math
import numpy as np
import concourse.bass as bass
import concourse.mybir as mybir
from concourse.bass_utils import run_bass_kernel_spmd

F32 = mybir.dt.float32
BF16 = mybir.dt.bfloat16
I32 = mybir.dt.int32
ALU = mybir.AluOpType
AF = mybir.ActivationFunctionType
AX = mybir.AxisListType

ENGS = ['sync', 'scalar', 'vector', 'gpsimd', 'tensor']
N_DMA_SEMS = 32
D = 1024
NCORES = 8
EPS = 1e-6
BIG = 1.0e30
import os as _os
DN_PHASE = float(_os.environ.get('DN_PHASE', '99'))


class Sched:
    def __init__(self, nc):
        self.nc = nc
        self.items = {e: [] for e in ENGS}
        self.cnt = {e: 0 for e in ENGS}
        self.seen = {e: {} for e in ENGS}
        self.lastw = {}
        self.readers = {}
        self.dma_rr = 0
        self.dma_cnt = [0] * N_DMA_SEMS
        self.pending = {e: [] for e in ENGS}
        self.out_tokens = []

    def _deps(self, reads, writes):
        toks = []
        for k in reads:
            t = self.lastw.get(k)
            if t is not None:
                toks.append(t)
        for k in writes:
            t = self.lastw.get(k)
            if t is not None:
                toks.append(t)
            toks.extend(self.readers.get(k, ()))
        return toks

    def _waits(self, eng, toks):
        need = {}
        for (sk, v) in toks:
            if sk == ('e', eng) and eng in ('tensor', 'sync'):
                continue
            if self.seen[eng].get(sk, 0) >= v:
                continue
            if need.get(sk, 0) < v:
                need[sk] = v
        for sk, v in need.items():
            self.seen[eng][sk] = v
        return list(need.items())

    def _commit(self, tok, reads, writes):
        for k in reads:
            self.readers.setdefault(k, []).append(tok)
        for k in writes:
            self.lastw[k] = tok
            self.readers[k] = []

    def op(self, eng, fn, reads=(), writes=()):
        toks = self._deps(reads, writes) + self.pending[eng]
        self.pending[eng] = []
        waits = self._waits(eng, toks)
        self.cnt[eng] += 1
        tok = (('e', eng), self.cnt[eng])
        self.items[eng].append((waits, fn, ('e', eng), 1))
        self._commit(tok, reads, writes)
        return tok

    def dma(self, eng, fn, reads=(), writes=(), is_output=False):
        toks = self._deps(reads, writes) + self.pending[eng]
        self.pending[eng] = []
        idx = self.dma_rr
        self.dma_rr = (self.dma_rr + 1) % N_DMA_SEMS
        if self.dma_cnt[idx] > 0:
            toks.append((('d', idx), self.dma_cnt[idx]))
        waits = self._waits(eng, toks)
        self.dma_cnt[idx] += 16
        tok = (('d', idx), self.dma_cnt[idx])
        self.items[eng].append((waits, fn, ('d', idx), 16))
        self._commit(tok, reads, writes)
        if is_output:
            self.out_tokens.append(tok)
        return tok

    def all_tokens(self):
        toks = []
        for d in range(N_DMA_SEMS):
            if self.dma_cnt[d] > 0:
                toks.append((('d', d), self.dma_cnt[d]))
        for e in ENGS:
            if self.cnt[e] > 0:
                toks.append((('e', e), self.cnt[e]))
        return toks

    def barrier(self):
        toks = self.all_tokens()
        for e in ENGS:
            self.pending[e] = self.pending[e] + toks

    def _ensure_sems(self):
        if getattr(self, 'semstack', None) is None:
            nc = self.nc
            self.semstack = contextlib.ExitStack()
            self.esem = {e: self.semstack.enter_context(nc.semaphore('se_' + e)) for e in ENGS}
            self.dsem = [self.semstack.enter_context(nc.semaphore('sd_%d' % i)) for i in range(N_DMA_SEMS)]

    def flush(self, final=False):
        nc = self.nc
        self._ensure_sems()
        final_waits = self._waits('sync', list(self.out_tokens) + self.all_tokens()) if final else []
        items = self.items
        self.items = {e: [] for e in ENGS}
        esem, dsem = self.esem, self.dsem

        def sem_of(sk):
            return esem[sk[1]] if sk[0] == 'e' else dsem[sk[1]]

        with nc.Block() as block:
            def run(engname, engobj):
                for (waits, fn, sk, inc) in items[engname]:
                    for (wsk, v) in waits:
                        engobj.wait_ge(sem_of(wsk), v)
                    fn(engobj).then_inc(sem_of(sk), inc)
                if engname == 'sync':
                    for (wsk, v) in final_waits:
                        engobj.wait_ge(sem_of(wsk), v)

            @block.sync
            def _(e):
                run('sync', e)

            @block.scalar
            def _(e):
                run('scalar', e)

            @block.vector
            def _(e):
                run('vector', e)

            @block.gpsimd
            def _(e):
                run('gpsimd', e)

            @block.tensor
            def _(e):
                run('tensor', e)
        if final:
            self.semstack.close()
            self.semstack = None

    def emit(self):
        self.flush(final=True)


class Ctx:
    def __init__(self, nc):
        self.nc = nc
        self.S = Sched(nc)
        self.uid = 0
        self.stack = contextlib.ExitStack()
        self.consts = {}

    def sb(self, shape, dt, name):
        self.uid += 1
        nm = '%s_%d' % (name, self.uid)
        t = self.stack.enter_context(self.nc.sbuf_tensor(nm, list(shape), dt))
        return t

    def ps(self, shape, dt, name):
        self.uid += 1
        nm = '%s_%d' % (name, self.uid)
        t = self.stack.enter_context(self.nc.psum_tensor(nm, list(shape), dt))
        return t

    @contextlib.contextmanager
    def scope(self):
        outer = self.stack
        self.stack = contextlib.ExitStack()
        try:
            yield
        finally:
            self.S.barrier()
            self.S.flush()
            self.stack.close()
            self.stack = outer


def K(*tiles):
    return [t if isinstance(t, (str, tuple)) else t.name for t in tiles]


def DK(ap, *idx):
    return ('dram', ap.name) + tuple(idx)


def host_consts():
    c = {}
    c['ident'] = np.eye(128, dtype=np.float32)
    i = np.arange(128)
    c['lstrict'] = (i[:, None] < i[None, :]).astype(np.float32)
    c['ones'] = np.ones((128, 128), dtype=np.float32)
    c['causT'] = (i[:, None] <= i[None, :]).astype(np.float32)
    return c


CONST_SHAPES = {'ident': [128, 128], 'lstrict': [128, 128], 'ones': [128, 128], 'causT': [128, 128]}


def load_consts(cx, dram):
    S = cx.S
    for nm in CONST_SHAPES:
        tf = cx.sb(CONST_SHAPES[nm], F32, 'c_' + nm)
        S.dma('sync', lambda e, tf=tf, nm=nm: e.dma_start(out=tf[:], in_=dram[nm]), writes=K(tf))
        tb = cx.sb(CONST_SHAPES[nm], BF16, 'cb_' + nm)
        S.op('vector', lambda e, tf=tf, tb=tb: e.tensor_copy(out=tb[:], in_=tf[:]), reads=K(tf), writes=K(tb))
        cx.consts[nm] = tf
        cx.consts[nm + '_bf'] = tb


def emit_mod(cx, cT_d, ada_w, ada_b, norm_mix, norm_ffn, mods):
    S = cx.S
    cT = cx.sb([128, 8], F32, 'cT')
    S.dma('sync', lambda e: e.dma_start(out=cT[:], in_=cT_d), writes=K(cT))
    cact = cx.sb([128, 8], F32, 'cact')
    S.op('scalar', lambda e: e.activation(out=cact[:], in_=cT[:], func=AF.Silu), reads=K(cT), writes=K(cact))
    cb = cx.sb([128, 8, 128], F32, 'cb')
    S.op('vector', lambda e: e.tensor_copy(out=cb[:], in_=cact[:].unsqueeze(2).to_broadcast([128, 8, 128])),
         reads=K(cact), writes=K(cb))
    gm = cx.sb([128, 1024], F32, 'gmix')
    gf = cx.sb([128, 1024], F32, 'gffn')
    S.dma('sync', lambda e: e.dma_start(out=gm[:], in_=norm_mix.partition_broadcast(128)), writes=K(gm))
    S.dma('sync', lambda e: e.dma_start(out=gf[:], in_=norm_ffn.partition_broadcast(128)), writes=K(gf))
    wbuf = [cx.sb([128, 8, 512], F32, 'adaw%d' % i) for i in range(2)]
    bbuf = [cx.sb([128, 512], F32, 'adab%d' % i) for i in range(2)]
    pm = [cx.ps([128, 512], F32, 'pmod%d' % i) for i in range(2)]
    order = ['Bm', 'Sm', 'Gm', 'Bf', 'Sf', 'Gf']
    sm = cx.sb([128, 1024], F32, 'scale_m')
    sf = cx.sb([128, 1024], F32, 'scale_f')
    dst = {'Bm': mods['Bm'], 'Sm': sm, 'Gm': mods['Gm'], 'Bf': mods['Bf'], 'Sf': sf, 'Gf': mods['Gf']}
    for j in range(12):
        b = j % 2
        S.dma('sync', lambda e, j=j, b=b: e.dma_start(out=wbuf[b][:], in_=ada_w[:, j * 512:(j + 1) * 512].rearrange("(k p) n -> p k n", p=128)),
              writes=K(wbuf[b]))
        S.dma('sync', lambda e, j=j, b=b: e.dma_start(out=bbuf[b][:], in_=ada_b[j * 512:(j + 1) * 512].partition_broadcast(128)),
              writes=K(bbuf[b]))
        for k in range(8):
            S.op('tensor', lambda e, k=k, b=b: e.matmul(pm[b][:], lhsT=cb[:, k, :], rhs=wbuf[b][:, k, :], start=(k == 0), stop=(k == 7)),
                 reads=K(cb, wbuf[b]), writes=K(pm[b]))
        d = dst[order[j // 2]]
        half = j % 2
        S.op('vector', lambda e, d=d, half=half, b=b: e.tensor_tensor(out=d[:, half * 512:(half + 1) * 512], in0=pm[b][:], in1=bbuf[b][:], op=ALU.add),
             reads=K(pm[b], bbuf[b]), writes=K(d))
    S.op('vector', lambda e: e.scalar_tensor_tensor(out=mods['Am'][:], in0=sm[:], scalar=1.0, in1=gm[:], op0=ALU.add, op1=ALU.mult),
         reads=K(sm, gm), writes=K(mods['Am']))
    S.op('vector', lambda e: e.scalar_tensor_tensor(out=mods['Af'][:], in0=sf[:], scalar=1.0, in1=gf[:], op0=ALU.add, op1=ALU.mult),
         reads=K(sf, gf), writes=K(mods['Af']))


def alloc_mods(cx):
    return {n: cx.sb([128, 1024], F32, 'mod' + n) for n in ['Am', 'Bm', 'Gm', 'Af', 'Bf', 'Gf']}


class NormBufs:
    def __init__(self, cx, tag):
        self.junk = cx.sb([128, 1024], F32, tag + 'junk')
        self.ss = cx.sb([128, 1], F32, tag + 'ss')
        self.rstd = cx.sb([128, 1], F32, tag + 'rstd')
        self.t = cx.sb([128, 1024], F32, tag + 'nt')


def emit_norm_mod(cx, nb, xt, A, B, outs):
    S = cx.S
    S.op('scalar', lambda e: e.activation(out=nb.junk[:], in_=xt[:], func=AF.Square, accum_out=nb.ss[:]),
         reads=K(xt), writes=K(nb.junk, nb.ss))
    S.op('scalar', lambda e: e.activation(out=nb.rstd[:], in_=nb.ss[:], func=AF.Sqrt, bias=EPS, scale=1.0 / D),
         reads=K(nb.ss), writes=K(nb.rstd))
    S.op('vector', lambda e: e.reciprocal(out=nb.rstd[:], in_=nb.rstd[:]), reads=K(nb.rstd), writes=K(nb.rstd))
    S.op('vector', lambda e: e.scalar_tensor_tensor(out=nb.t[:], in0=xt[:], scalar=nb.rstd[:, 0:1], in1=A[:], op0=ALU.mult, op1=ALU.mult),
         reads=K(xt, nb.rstd, A), writes=K(nb.t))
    for i, o in enumerate(outs):
        eng = 'vector' if i == 0 else 'gpsimd'
        S.op(eng, lambda e, o=o: e.tensor_tensor(out=o[:], in0=nb.t[:], in1=B[:], op=ALU.add), reads=K(nb.t, B), writes=K(o))


def emit_transposes(cx, src, nchunk, pT, dstT, ident, copy_eng='scalar'):
    S = cx.S
    for k in range(nchunk):
        S.op('tensor', lambda e, k=k: e.transpose(out=pT[:, k, :], in_=src[:, k * 128:(k + 1) * 128], identity=ident[:]),
             reads=K(src, ident), writes=K(pT))
    if copy_eng == 'scalar':
        S.op('scalar', lambda e: e.copy(out=dstT[:, 0:nchunk, :], in_=pT[:, 0:nchunk, :]), reads=K(pT), writes=K(dstT))
    else:
        S.op('vector', lambda e: e.tensor_copy(out=dstT[:, 0:nchunk, :], in_=pT[:, 0:nchunk, :]), reads=K(pT), writes=K(dstT))


def emit_gmlp(cx, x_in, x_out, ntiles, mods, w_in, v_norm, w_s, b_sT, w_out):
    S = cx.S
    idb = cx.consts['ident_bf']
    idf = cx.consts['ident']
    wi = cx.sb([128, 8, 2048], BF16, 'gm_wi')
    wo = cx.sb([128, 8, 1024], BF16, 'gm_wo')
    for k in range(8):
        S.dma('gpsimd', lambda e, k=k: e.dma_start(out=wi[:, k, :], in_=w_in[k * 128:(k + 1) * 128, :]), writes=K(wi))
    for k in range(8):
        S.dma('gpsimd', lambda e, k=k: e.dma_start(out=wo[:, k, :], in_=w_out[k * 128:(k + 1) * 128, :]), writes=K(wo))
    vg = cx.sb([128, 1024], F32, 'gm_vg')
    S.dma('sync', lambda e: e.dma_start(out=vg[:], in_=v_norm.partition_broadcast(128)), writes=K(vg))
    bsT = cx.sb([128, 8], F32, 'gm_bsT')
    S.dma('sync', lambda e: e.dma_start(out=bsT[:], in_=b_sT), writes=K(bsT))
    wsf = cx.sb([128, 8, 128], F32, 'gm_wsf')
    S.dma('sync', lambda e: e.dma_start(out=wsf[:], in_=w_s.rearrange("g t s -> t g s")), writes=K(wsf))
    psv = cx.ps([128, 1024], F32, 'gm_psv')
    for g in range(8):
        S.op('tensor', lambda e, g=g: e.transpose(out=psv[:, g * 128:(g + 1) * 128], in_=wsf[:, g, :], identity=idf[:]), reads=K(wsf, idf), writes=K(psv))
    WcT = cx.sb([128, 8, 128], BF16, 'gm_WcT')
    causT = cx.consts['causT']
    S.op('vector', lambda e: e.tensor_tensor(out=WcT[:], in0=psv[:].rearrange("p (g t) -> p g t", g=8), in1=causT[:].unsqueeze(1).to_broadcast([128, 8, 128]), op=ALU.mult),
         reads=K(psv, causT), writes=K(WcT))

    nbuf = NormBufs(cx, 'gm')
    xts = [cx.sb([128, 1024], F32, 'gm_x%d' % i) for i in range(2)]
    h = cx.sb([128, 1024], BF16, 'gm_h')
    hT = cx.sb([128, 8, 128], BF16, 'gm_hT')
    pT = cx.ps([128, 8, 128], BF16, 'gm_pT')
    pz = [cx.ps([128, 512], F32, 'gm_pz%d' % i) for i in range(4)]
    u = cx.sb([128, 1024], BF16, 'gm_u')
    v = cx.sb([128, 1024], F32, 'gm_v')
    vn = cx.sb([128, 1024], BF16, 'gm_vn')
    m = cx.sb([128, 1024], BF16, 'gm_m')
    mT = cx.sb([128, 8, 128], BF16, 'gm_mT')
    ssv = cx.sb([128, 1], F32, 'gm_ssv')
    rsv = cx.sb([128, 1], F32, 'gm_rsv')
    t1 = cx.sb([128, 1024], F32, 'gm_t1')
    xo = [cx.sb([128, 1024], F32, 'gm_xo%d' % i) for i in range(2)]

    for t in range(ntiles):
        xt = xts[t % 2]
        S.dma('sync', lambda e, t=t, xt=xt: e.dma_start(out=xt[:], in_=x_in[t * 128:(t + 1) * 128, :]), reads=[DK(x_in, t)], writes=K(xt))
        emit_norm_mod(cx, nbuf, xt, mods['Am'], mods['Bm'], [h])
        emit_transposes(cx, h, 8, pT, hT, idb)
        for n in range(4):
            for k in range(8):
                S.op('tensor', lambda e, n=n, k=k: e.matmul(pz[n][:], lhsT=hT[:, k, :], rhs=wi[:, k, n * 512:(n + 1) * 512], start=(k == 0), stop=(k == 7)),
                     reads=K(hT, wi), writes=K(pz[n]))
        for n in range(2):
            S.op('scalar', lambda e, n=n: e.activation(out=u[:, n * 512:(n + 1) * 512], in_=pz[n][:], func=AF.Gelu_apprx_tanh),
                 reads=K(pz[n]), writes=K(u))
        for n in range(2):
            S.op('scalar', lambda e, n=n: e.activation(out=v[:, n * 512:(n + 1) * 512], in_=pz[2 + n][:], func=AF.Gelu_apprx_tanh),
                 reads=K(pz[2 + n]), writes=K(v))
        S.op('scalar', lambda e: e.activation(out=nbuf.junk[:], in_=v[:], func=AF.Square, accum_out=ssv[:]),
             reads=K(v), writes=K(nbuf.junk, ssv))
        S.op('scalar', lambda e: e.activation(out=rsv[:], in_=ssv[:], func=AF.Sqrt, bias=EPS, scale=1.0 / 1024), reads=K(ssv), writes=K(rsv))
        S.op('vector', lambda e: e.reciprocal(out=rsv[:], in_=rsv[:]), reads=K(rsv), writes=K(rsv))
        S.op('vector', lambda e: e.scalar_tensor_tensor(out=vn[:], in0=v[:], scalar=rsv[:, 0:1], in1=vg[:], op0=ALU.mult, op1=ALU.mult),
             reads=K(v, rsv, vg), writes=K(vn))
        for g in range(8):
            S.op('tensor', lambda e, g=g: e.matmul(psv[:, g * 128:(g + 1) * 128], lhsT=WcT[:, g, :], rhs=vn[:, g * 128:(g + 1) * 128], start=True, stop=True),
                 reads=K(WcT, vn), writes=K(psv))
        for g in range(8):
            S.op('vector', lambda e, g=g: e.scalar_tensor_tensor(out=m[:, g * 128:(g + 1) * 128], in0=psv[:, g * 128:(g + 1) * 128], scalar=bsT[:, g:g + 1],
                                                                in1=u[:, g * 128:(g + 1) * 128], op0=ALU.add, op1=ALU.mult),
                 reads=K(psv, bsT, u), writes=K(m))
        emit_transposes(cx, m, 8, pT, mT, idb)
        for n in range(2):
            for k in range(8):
                S.op('tensor', lambda e, n=n, k=k: e.matmul(pz[n][:], lhsT=mT[:, k, :], rhs=wo[:, k, n * 512:(n + 1) * 512], start=(k == 0), stop=(k == 7)),
                     reads=K(mT, wo), writes=K(pz[n]))
        o = xo[t % 2]
        for n in range(2):
            S.op('vector', lambda e, n=n: e.tensor_tensor(out=t1[:, n * 512:(n + 1) * 512], in0=pz[n][:], in1=mods['Gm'][:, n * 512:(n + 1) * 512], op=ALU.mult),
                 reads=K(pz[n], mods['Gm']), writes=K(t1))
        S.op('gpsimd', lambda e, o=o, xt=xt: e.tensor_tensor(out=o[:], in0=t1[:], in1=xt[:], op=ALU.add), reads=K(t1, xt), writes=K(o))
        S.dma('sync', lambda e, t=t, o=o: e.dma_start(out=x_out[t * 128:(t + 1) * 128, :], in_=o[:]), reads=K(o), writes=[DK(x_out, t)], is_output=True)


def moe_host_consts(NT):
    NB = 2 * NT + 32
    c = {}
    c['thr1'] = np.broadcast_to((128.0 * np.arange(NT, dtype=np.float32))[None, None, :], (128, 32, NT)).reshape(128, 32 * NT).copy()
    c['thr2'] = np.broadcast_to((128.0 * np.arange(NB, dtype=np.float32))[None, :, None], (128, NB, 32)).reshape(128, NB * 32).copy()
    p = np.arange(128, dtype=np.float32)[:, None]
    c['wbi'] = (np.arange(8, dtype=np.float32)[None, :] * 128 + p).astype(np.float32)
    c['wbo'] = (np.arange(4, dtype=np.float32)[None, :] * 128 + p).astype(np.float32)
    return c


def moe_const_shapes(NT):
    NB = 2 * NT + 32
    return {'thr1': [128, 32 * NT], 'thr2': [128, NB * 32], 'wbi': [128, 8], 'wbo': [128, 4]}


def emit_moe(cx, x_in, x_out, NT, mods, router_w, router_b, ew_in, ew_out, mc, scr):
    S = cx.S
    NB = 2 * NT + 32
    idb = cx.consts['ident_bf']
    idf = cx.consts['ident']
    lsb = cx.consts['lstrict_bf']
    onb = cx.consts['ones_bf']
    hs, xpad, ys = scr['hs'], scr['xpad'], scr['ys']
    wrows_in = ew_in.rearrange("e d n -> (e d) n")
    wrows_out = ew_out.rearrange("e f n -> (e f) n")

    g0s = cx.sb([128, NT], F32, 'mo_g0s')
    g1s = cx.sb([128, NT], F32, 'mo_g1s')
    dis = [cx.sb([128, NT], I32, 'mo_d0i'), cx.sb([128, NT], I32, 'mo_d1i')]
    wii = cx.sb([128, NB, 8], I32, 'mo_wii')
    woi = cx.sb([128, NB, 4], I32, 'mo_woi')
    xts = [cx.sb([128, 1024], F32, 'mo_x%d' % i) for i in range(2)]
    hb = [cx.sb([128, 1024], BF16, 'mo_hb%d' % i) for i in range(2)]
    scope1 = cx.scope()
    scope1.__enter__()
    rw = cx.sb([128, 8, 36], F32, 'mo_rw')
    S.dma('sync', lambda e: e.dma_start(out=rw[:], in_=router_w.rearrange("(k p) n -> p k n", p=128)), writes=K(rw))
    rb = cx.sb([128, 36], F32, 'mo_rb')
    S.dma('sync', lambda e: e.dma_start(out=rb[:], in_=router_b.partition_broadcast(128)), writes=K(rb))
    thr1 = cx.sb([128, 32, NT], F32, 'mo_thr1')
    S.dma('sync', lambda e: e.dma_start(out=thr1[:], in_=mc['thr1'].rearrange("p (a b) -> p a b", a=32)), writes=K(thr1))
    thr2 = cx.sb([128, NB, 32], F32, 'mo_thr2')
    S.dma('sync', lambda e: e.dma_start(out=thr2[:], in_=mc['thr2'].rearrange("p (a b) -> p a b", a=NB)), writes=K(thr2))
    wbi = cx.sb([128, 8], F32, 'mo_wbi')
    S.dma('sync', lambda e: e.dma_start(out=wbi[:], in_=mc['wbi']), writes=K(wbi))
    wbo = cx.sb([128, 4], F32, 'mo_wbo')
    S.dma('sync', lambda e: e.dma_start(out=wbo[:], in_=mc['wbo']), writes=K(wbo))

    oh1s = cx.sb([128, NT, 32], F32, 'mo_oh1s')
    oh2s = cx.sb([128, NT, 32], F32, 'mo_oh2s')
    rks = cx.sb([128, NT, 32], F32, 'mo_rks')
    run = cx.sb([128, 32], F32, 'mo_run')
    S.op('vector', lambda e: e.memset(run[:], 0.0), writes=K(run))

    nbuf = NormBufs(cx, 'mo')
    hf = cx.sb([128, 1024], F32, 'mo_hf')
    hTf = cx.sb([128, 8, 128], F32, 'mo_hTf')
    pTf = cx.ps([128, 8, 128], F32, 'mo_pTf')
    psm = cx.ps([128, 3, 64], F32, 'mo_psm')

    def sm(name, w=1):
        return cx.sb([128, w], F32, 'mo_' + name)
    lg = sm('lg', 36); gmax = sm('gmax'); ohg = sm('ohg', 4); negg = sm('negg'); eg = sm('eg', 4); se = sm('se'); pg = sm('pg')
    pen = sm('pen', 4); ml = sm('ml', 32); m1 = sm('m1'); ml2 = sm('ml2', 32); m2 = sm('m2'); dd = sm('dd'); e2 = sm('e2')
    den = sm('den'); rr = sm('rr')
    Mb = cx.sb([128, 32], BF16, 'mo_Mb')

    for t in range(NT):
        xt = xts[t % 2]
        hbt = hb[t % 2]
        S.dma('sync', lambda e, t=t, xt=xt: e.dma_start(out=xt[:], in_=x_in[t * 128:(t + 1) * 128, :]), reads=[DK(x_in, t)], writes=K(xt))
        emit_norm_mod(cx, nbuf, xt, mods['Af'], mods['Bf'], [hf, hbt])
        S.dma('sync', lambda e, t=t, hbt=hbt: e.dma_start(out=hs[t * 128:(t + 1) * 128, :], in_=hbt[:]), reads=K(hbt), writes=[DK(hs, t)])
        emit_transposes(cx, hf, 8, pTf, hTf, idf)
        for k in range(8):
            S.op('tensor', lambda e, k=k: e.matmul(psm[:, 0, 0:36], lhsT=hTf[:, k, :], rhs=rw[:, k, :], start=(k == 0), stop=(k == 7)),
                 reads=K(hTf, rw), writes=[(psm.name, 0)])
        S.op('vector', lambda e: e.tensor_tensor(out=lg[:], in0=psm[:, 0, 0:36], in1=rb[:], op=ALU.add), reads=[(psm.name, 0)] + K(rb), writes=K(lg))
        S.op('vector', lambda e: e.tensor_reduce(out=gmax[:], in_=lg[:, 0:4], axis=AX.X, op=ALU.max), reads=K(lg), writes=K(gmax))
        S.op('vector', lambda e: e.tensor_scalar(out=ohg[:], in0=lg[:, 0:4], scalar1=gmax[:, 0:1], scalar2=None, op0=ALU.is_equal), reads=K(lg, gmax), writes=K(ohg))
        S.op('vector', lambda e: e.tensor_scalar(out=negg[:], in0=gmax[:], scalar1=-1.0, scalar2=None, op0=ALU.mult), reads=K(gmax), writes=K(negg))
        S.op('scalar', lambda e: e.activation(out=eg[:], in_=lg[:, 0:4], func=AF.Exp, bias=negg[:, 0:1], scale=1.0, accum_out=se[:]),
             reads=K(lg, negg), writes=K(eg, se))
        S.op('vector', lambda e: e.reciprocal(out=pg[:], in_=se[:]), reads=K(se), writes=K(pg))
        S.op('vector', lambda e: e.tensor_scalar(out=pen[:], in0=ohg[:], scalar1=BIG, scalar2=-BIG, op0=ALU.mult, op1=ALU.add), reads=K(ohg), writes=K(pen))
        S.op('vector', lambda e: e.tensor_tensor(out=ml[:].rearrange("p (g j) -> p g j", g=4), in0=lg[:, 4:36].rearrange("p (g j) -> p g j", g=4),
                                                in1=pen[:].unsqueeze(2).to_broadcast([128, 4, 8]), op=ALU.add), reads=K(lg, pen), writes=K(ml))
        S.op('vector', lambda e: e.tensor_reduce(out=m1[:], in_=ml[:], axis=AX.X, op=ALU.max), reads=K(ml), writes=K(m1))
        o1 = oh1s[:, t, :]
        o2 = oh2s[:, t, :]
        S.op('vector', lambda e, o1=o1: e.tensor_scalar(out=o1, in0=ml[:], scalar1=m1[:, 0:1], scalar2=None, op0=ALU.is_equal), reads=K(ml, m1), writes=K(oh1s))
        S.op('vector', lambda e, o1=o1: e.scalar_tensor_tensor(out=ml2[:], in0=o1, scalar=-BIG, in1=ml[:], op0=ALU.mult, op1=ALU.add), reads=K(oh1s, ml), writes=K(ml2))
        S.op('vector', lambda e: e.tensor_reduce(out=m2[:], in_=ml2[:], axis=AX.X, op=ALU.max), reads=K(ml2), writes=K(m2))
        S.op('vector', lambda e, o2=o2: e.tensor_scalar(out=o2, in0=ml2[:], scalar1=m2[:, 0:1], scalar2=None, op0=ALU.is_equal), reads=K(ml2, m2), writes=K(oh2s))
        S.op('vector', lambda e: e.tensor_tensor(out=dd[:], in0=m2[:], in1=m1[:], op=ALU.subtract), reads=K(m1, m2), writes=K(dd))
        S.op('scalar', lambda e: e.activation(out=e2[:], in_=dd[:], func=AF.Exp), reads=K(dd), writes=K(e2))
        S.op('vector', lambda e: e.tensor_scalar(out=den[:], in0=e2[:], scalar1=1.0, scalar2=None, op0=ALU.add), reads=K(e2), writes=K(den))
        S.op('vector', lambda e: e.reciprocal(out=rr[:], in_=den[:]), reads=K(den), writes=K(rr))
        S.op('vector', lambda e, t=t: e.tensor_tensor(out=g0s[:, t:t + 1], in0=pg[:], in1=rr[:], op=ALU.mult), reads=K(pg, rr), writes=K(g0s))
        S.op('vector', lambda e, t=t: e.tensor_tensor(out=g1s[:, t:t + 1], in0=g0s[:, t:t + 1], in1=e2[:], op=ALU.mult), reads=K(g0s, e2), writes=K(g1s))
        S.op('vector', lambda e, o1=o1, o2=o2: e.tensor_tensor(out=Mb[:], in0=o1, in1=o2, op=ALU.add), reads=K(oh1s, oh2s), writes=K(Mb))
        S.op('tensor', lambda e: e.matmul(psm[:, 1, 0:32], lhsT=lsb[:], rhs=Mb[:], start=True, stop=True), reads=K(lsb, Mb), writes=[(psm.name, 1)])
        S.op('tensor', lambda e: e.matmul(psm[:, 2, 0:32], lhsT=onb[:], rhs=Mb[:], start=True, stop=True), reads=K(onb, Mb), writes=[(psm.name, 2)])
        S.op('vector', lambda e, t=t: e.tensor_tensor(out=rks[:, t, :], in0=psm[:, 1, 0:32], in1=run[:], op=ALU.add), reads=[(psm.name, 1)] + K(run), writes=K(rks))
        S.op('vector', lambda e: e.tensor_tensor(out=run[:], in0=psm[:, 2, 0:32], in1=run[:], op=ALU.add), reads=[(psm.name, 2)] + K(run), writes=K(run))

    cmp1 = cx.sb([128, 32, NT], F32, 'mo_cmp1')
    S.op('vector', lambda e: e.tensor_tensor(out=cmp1[:], in0=run[:].unsqueeze(2).to_broadcast([128, 32, NT]), in1=thr1[:], op=ALU.is_gt),
         reads=K(run, thr1), writes=K(cmp1))
    padded = sm('padded', 32)
    S.op('vector', lambda e: e.tensor_reduce(out=padded[:], in_=cmp1[:], axis=AX.X, op=ALU.add), reads=K(cmp1), writes=K(padded))
    S.op('vector', lambda e: e.tensor_scalar(out=padded[:], in0=padded[:], scalar1=128.0, scalar2=None, op0=ALU.mult), reads=K(padded), writes=K(padded))
    cs = [sm('cs%d' % i, 32) for i in range(2)]
    S.op('vector', lambda e: e.tensor_copy(out=cs[0][:], in_=padded[:]), reads=K(padded), writes=K(cs[0]))
    cur = 0
    for s in (1, 2, 4, 8, 16):
        a, b = cs[cur], cs[1 - cur]
        S.op('vector', lambda e, a=a, b=b, s=s: e.tensor_copy(out=b[:, 0:s], in_=a[:, 0:s]), reads=K(a), writes=K(b))
        S.op('vector', lambda e, a=a, b=b, s=s: e.tensor_tensor(out=b[:, s:32], in0=a[:, s:32], in1=a[:, 0:32 - s], op=ALU.add), reads=K(a), writes=K(b))
        cur = 1 - cur
    pend = cs[cur]
    pstart = sm('pstart', 32)
    S.op('vector', lambda e: e.tensor_tensor(out=pstart[:], in0=pend[:], in1=padded[:], op=ALU.subtract), reads=K(pend, padded), writes=K(pstart))
    tmp = cx.sb([128, NT, 32], F32, 'mo_tmp')
    tmp2 = cx.sb([128, NT, 32], F32, 'mo_tmp2')
    S.op('vector', lambda e: e.tensor_tensor(out=tmp[:], in0=rks[:], in1=pstart[:].unsqueeze(1).to_broadcast([128, NT, 32]), op=ALU.add),
         reads=K(rks, pstart), writes=K(tmp))
    dfs = [sm('d0f', NT), sm('d1f', NT)]
    for i, ohs in enumerate((oh1s, oh2s)):
        S.op('vector', lambda e, ohs=ohs: e.tensor_tensor(out=tmp2[:], in0=tmp[:], in1=ohs[:], op=ALU.mult), reads=K(tmp, ohs), writes=K(tmp2))
        S.op('vector', lambda e, i=i: e.tensor_reduce(out=dfs[i][:], in_=tmp2[:], axis=AX.X, op=ALU.add), reads=K(tmp2), writes=K(dfs[i]))
        S.op('vector', lambda e, i=i: e.tensor_copy(out=dis[i][:], in_=dfs[i][:]), reads=K(dfs[i]), writes=K(dis[i]))
    cmp2 = cx.sb([128, NB, 32], F32, 'mo_cmp2')
    S.op('vector', lambda e: e.tensor_tensor(out=cmp2[:], in0=pend[:].unsqueeze(1).to_broadcast([128, NB, 32]), in1=thr2[:], op=ALU.is_le),
         reads=K(pend, thr2), writes=K(cmp2))
    be = sm('be', NB)
    S.op('vector', lambda e: e.tensor_reduce(out=be[:], in_=cmp2[:], axis=AX.X, op=ALU.add), reads=K(cmp2), writes=K(be))
    S.op('vector', lambda e: e.tensor_scalar(out=be[:], in0=be[:], scalar1=31.0, scalar2=None, op0=ALU.min), reads=K(be), writes=K(be))
    wif = cx.sb([128, NB, 8], F32, 'mo_wif')
    wof = cx.sb([128, NB, 4], F32, 'mo_wof')
    S.op('vector', lambda e: e.tensor_scalar(out=wif[:], in0=be[:].unsqueeze(2).to_broadcast([128, NB, 8]), scalar1=1024.0, scalar2=None, op0=ALU.mult), reads=K(be), writes=K(wif))
    S.op('vector', lambda e: e.tensor_tensor(out=wif[:], in0=wif[:], in1=wbi[:].unsqueeze(1).to_broadcast([128, NB, 8]), op=ALU.add), reads=K(wif, wbi), writes=K(wif))
    S.op('vector', lambda e: e.tensor_copy(out=wii[:], in_=wif[:]), reads=K(wif), writes=K(wii))
    S.op('vector', lambda e: e.tensor_scalar(out=wof[:], in0=be[:].unsqueeze(2).to_broadcast([128, NB, 4]), scalar1=512.0, scalar2=None, op0=ALU.mult), reads=K(be), writes=K(wof))
    S.op('vector', lambda e: e.tensor_tensor(out=wof[:], in0=wof[:], in1=wbo[:].unsqueeze(1).to_broadcast([128, NB, 4]), op=ALU.add), reads=K(wof, wbo), writes=K(wof))
    S.op('vector', lambda e: e.tensor_copy(out=woi[:], in_=wof[:]), reads=K(wof), writes=K(woi))

    scope1.__exit__(None, None, None)
    scope2 = cx.scope()
    scope2.__enter__()
    pTb = cx.ps([128, 8, 128], BF16, 'mo_pTb')
    pag = [cx.ps([128, 512], F32, 'mo_pag%d' % i) for i in range(2)]
    py = [cx.ps([128, 512], F32, 'mo_py%d' % i) for i in range(2)]
    for t in range(NT):
        hbt = hb[t % 2]
        S.dma('sync', lambda e, t=t, hbt=hbt: e.dma_start(out=hbt[:], in_=hs[t * 128:(t + 1) * 128, :]), reads=[DK(hs, t)], writes=K(hbt))
        for i in range(2):
            S.dma('gpsimd', lambda e, t=t, i=i, hbt=hbt: e.indirect_dma_start(out=xpad, out_offset=bass.IndirectOffsetOnAxis(ap=dis[i][:, t:t + 1], axis=0),
                                                                             in_=hbt[:], in_offset=None),
                  reads=K(hbt, dis[i]), writes=[DK(xpad, t, i)])
    xpad_keys = [DK(xpad, t, i) for t in range(NT) for i in range(2)]

    wi = [cx.sb([128, 8, 1024], BF16, 'mo_wi%d' % i) for i in range(2)]
    wo = [cx.sb([128, 4, 1024], BF16, 'mo_wo%d' % i) for i in range(2)]
    xb = [cx.sb([128, 1024], BF16, 'mo_xb%d' % i) for i in range(2)]
    xbT = cx.sb([128, 8, 128], BF16, 'mo_xbT')
    sa = cx.sb([128, 512], F32, 'mo_sa')
    act = cx.sb([128, 512], BF16, 'mo_act')
    actT = cx.sb([128, 4, 128], BF16, 'mo_actT')
    yb = [cx.sb([128, 1024], F32, 'mo_yb%d' % i) for i in range(2)]
    for k in range(NB):
        b = k % 2
        for j in range(8):
            S.dma('gpsimd', lambda e, k=k, j=j, b=b: e.indirect_dma_start(out=wi[b][:, j, :], out_offset=None, in_=wrows_in,
                                                                          in_offset=bass.IndirectOffsetOnAxis(ap=wii[:, k, j:j + 1], axis=0)),
                  reads=K(wii), writes=[(wi[b].name, j)])
        for j in range(4):
            S.dma('gpsimd', lambda e, k=k, j=j, b=b: e.indirect_dma_start(out=wo[b][:, j, :], out_offset=None, in_=wrows_out,
                                                                          in_offset=bass.IndirectOffsetOnAxis(ap=woi[:, k, j:j + 1], axis=0)),
                  reads=K(woi), writes=[(wo[b].name, j)])
        S.dma('sync', lambda e, k=k, b=b: e.dma_start(out=xb[b][:], in_=xpad[k * 128:(k + 1) * 128, :]), reads=xpad_keys, writes=K(xb[b]))
        emit_transposes(cx, xb[b], 8, pTb, xbT, idb)
        for n in range(2):
            for j in range(8):
                S.op('tensor', lambda e, n=n, j=j, b=b: e.matmul(pag[n][:], lhsT=xbT[:, j, :], rhs=wi[b][:, j, n * 512:(n + 1) * 512], start=(j == 0), stop=(j == 7)),
                     reads=K(xbT) + [(wi[b].name, j)], writes=K(pag[n]))
        S.op('scalar', lambda e: e.activation(out=sa[:], in_=pag[0][:], func=AF.Silu), reads=K(pag[0]), writes=K(sa))
        S.op('vector', lambda e: e.tensor_tensor(out=act[:], in0=sa[:], in1=pag[1][:], op=ALU.mult), reads=K(sa, pag[1]), writes=K(act))
        emit_transposes(cx, act, 4, pTb, actT, idb)
        for n in range(2):
            for j in range(4):
                S.op('tensor', lambda e, n=n, j=j, b=b: e.matmul(py[n][:], lhsT=actT[:, j, :], rhs=wo[b][:, j, n * 512:(n + 1) * 512], start=(j == 0), stop=(j == 3)),
                     reads=K(actT) + [(wo[b].name, j)], writes=K(py[n]))
        S.op('scalar', lambda e, b=b: e.copy(out=yb[b][:, 0:512], in_=py[0][:]), reads=K(py[0]), writes=[(yb[b].name, 0)])
        S.op('vector', lambda e, b=b: e.tensor_copy(out=yb[b][:, 512:1024], in_=py[1][:]), reads=K(py[1]), writes=[(yb[b].name, 1)])
        S.dma('sync', lambda e, k=k, b=b: e.dma_start(out=ys[k * 128:(k + 1) * 128, :], in_=yb[b][:]), reads=[(yb[b].name, 0), (yb[b].name, 1)], writes=[DK(ys, k)])
    ys_keys = [DK(ys, k) for k in range(NB)]

    y0 = [cx.sb([128, 1024], F32, 'mo_y0%d' % i) for i in range(2)]
    y1 = [cx.sb([128, 1024], F32, 'mo_y1%d' % i) for i in range(2)]
    acc = cx.sb([128, 1024], F32, 'mo_acc')
    xo = [cx.sb([128, 1024], F32, 'mo_xo%d' % i) for i in range(2)]
    for t in range(NT):
        b = t % 2
        xt = xts[b]
        S.dma('sync', lambda e, t=t, xt=xt: e.dma_start(out=xt[:], in_=x_in[t * 128:(t + 1) * 128, :]), reads=[DK(x_in, t)], writes=K(xt))
        S.dma('gpsimd', lambda e, t=t, b=b: e.indirect_dma_start(out=y0[b][:], out_offset=None, in_=ys, in_offset=bass.IndirectOffsetOnAxis(ap=dis[0][:, t:t + 1], axis=0)),
              reads=ys_keys + K(dis[0]), writes=K(y0[b]))
        S.dma('gpsimd', lambda e, t=t, b=b: e.indirect_dma_start(out=y1[b][:], out_offset=None, in_=ys, in_offset=bass.IndirectOffsetOnAxis(ap=dis[1][:, t:t + 1], axis=0)),
              reads=ys_keys + K(dis[1]), writes=K(y1[b]))
        S.op('vector', lambda e, t=t, b=b: e.tensor_scalar(out=acc[:], in0=y0[b][:], scalar1=g0s[:, t:t + 1], scalar2=None, op0=ALU.mult), reads=K(y0[b], g0s), writes=K(acc))
        S.op('vector', lambda e, t=t, b=b: e.scalar_tensor_tensor(out=acc[:], in0=y1[b][:], scalar=g1s[:, t:t + 1], in1=acc[:], op0=ALU.mult, op1=ALU.add),
             reads=K(y1[b], g1s, acc), writes=K(acc))
        S.op('gpsimd', lambda e: e.tensor_tensor(out=acc[:], in0=acc[:], in1=mods['Gf'][:], op=ALU.mult), reads=K(acc, mods['Gf']), writes=K(acc))
        S.op('vector', lambda e, b=b, xt=xt: e.tensor_tensor(out=xo[b][:], in0=acc[:], in1=xt[:], op=ALU.add), reads=K(acc, xt), writes=K(xo[b]))
        S.dma('sync', lambda e, t=t, b=b: e.dma_start(out=x_out[t * 128:(t + 1) * 128, :], in_=xo[b][:]), reads=K(xo[b]), writes=[DK(x_out, t)], is_output=True)
    scope2.__exit__(None, None, None)


def emit_swa(cx, x_in, x_out, NT, mods, w_in, q_norm, k_norm, sinks, w_out, x_halo, mprev0_d, gtT_d):
    S = cx.S
    idb = cx.consts['ident_bf']
    causb = cx.consts['causT_bf']
    wi = cx.sb([128, 8, 1536], BF16, 'sw_wi')
    wo = cx.sb([128, 8, 1024], BF16, 'sw_wo')
    for k in range(8):
        S.dma('gpsimd', lambda e, k=k: e.dma_start(out=wi[:, k, :], in_=w_in[k * 128:(k + 1) * 128, :]), writes=K(wi))
    for k in range(8):
        S.dma('gpsimd', lambda e, k=k: e.dma_start(out=wo[:, k, :], in_=w_out[k * 128:(k + 1) * 128, :]), writes=K(wo))
    gq = cx.sb([128, 64], F32, 'sw_gq')
    gk = cx.sb([128, 64], F32, 'sw_gk')
    sk = cx.sb([128, 16], F32, 'sw_sk')
    S.dma('sync', lambda e: e.dma_start(out=gq[:], in_=q_norm.partition_broadcast(128)), writes=K(gq))
    S.dma('sync', lambda e: e.dma_start(out=gk[:], in_=k_norm.partition_broadcast(128)), writes=K(gk))
    S.dma('sync', lambda e: e.dma_start(out=sk[:], in_=sinks.partition_broadcast(128)), writes=K(sk))
    mp0f = cx.sb([128, 128], F32, 'sw_mp0f')
    gtf = cx.sb([128, 128], F32, 'sw_gtf')
    S.dma('sync', lambda e: e.dma_start(out=mp0f[:], in_=mprev0_d), writes=K(mp0f))
    S.dma('sync', lambda e: e.dma_start(out=gtf[:], in_=gtT_d), writes=K(gtf))
    mp0 = cx.sb([128, 128], BF16, 'sw_mp0')
    gtb = cx.sb([128, 128], BF16, 'sw_gtb')
    S.op('vector', lambda e: e.tensor_copy(out=mp0[:], in_=mp0f[:]), reads=K(mp0f), writes=K(mp0))
    S.op('vector', lambda e: e.tensor_copy(out=gtb[:], in_=gtf[:]), reads=K(gtf), writes=K(gtb))

    def sm(name, w=1):
        return cx.sb([128, w], F32, 'sw_' + name)
    t64 = sm('t64', 64); mq = sm('mq'); mk = sm('mk'); bnd = sm('bnd'); msk = sm('msk'); cc = sm('cc'); negc = sm('negc'); esink = sm('esink', 16)
    S.op('vector', lambda e: e.tensor_tensor(out=t64[:], in0=gq[:], in1=gq[:], op=ALU.mult), reads=K(gq), writes=K(t64))
    S.op('vector', lambda e: e.tensor_reduce(out=mq[:], in_=t64[:], axis=AX.X, op=ALU.max), reads=K(t64), writes=K(mq))
    S.op('vector', lambda e: e.tensor_tensor(out=t64[:], in0=gk[:], in1=gk[:], op=ALU.mult), reads=K(gk, t64), writes=K(t64))
    S.op('vector', lambda e: e.tensor_reduce(out=mk[:], in_=t64[:], axis=AX.X, op=ALU.max), reads=K(t64), writes=K(mk))
    S.op('vector', lambda e: e.tensor_tensor(out=bnd[:], in0=mq[:], in1=mk[:], op=ALU.mult), reads=K(mq, mk), writes=K(bnd))
    S.op('scalar', lambda e: e.activation(out=bnd[:], in_=bnd[:], func=AF.Sqrt, scale=64.0), reads=K(bnd), writes=K(bnd))
    S.op('vector', lambda e: e.tensor_reduce(out=msk[:], in_=sk[:], axis=AX.X, op=ALU.max), reads=K(sk), writes=K(msk))
    S.op('vector', lambda e: e.tensor_tensor(out=cc[:], in0=bnd[:], in1=msk[:], op=ALU.max), reads=K(bnd, msk), writes=K(cc))
    S.op('vector', lambda e: e.tensor_scalar(out=negc[:], in0=cc[:], scalar1=-1.0, scalar2=None, op0=ALU.mult), reads=K(cc), writes=K(negc))
    S.op('scalar', lambda e: e.activation(out=esink[:], in_=sk[:], func=AF.Exp, bias=negc[:, 0:1], scale=1.0), reads=K(sk, negc), writes=K(esink))
    gq8 = sm('gq8', 64)
    S.op('vector', lambda e: e.tensor_scalar(out=gq8[:], in0=gq[:], scalar1=0.125, scalar2=None, op0=ALU.mult), reads=K(gq), writes=K(gq8))

    nbuf = NormBufs(cx, 'sw')
    xts = [cx.sb([128, 1024], F32, 'sw_x%d' % i) for i in range(2)]
    h = cx.sb([128, 1024], BF16, 'sw_h')
    hT = cx.sb([128, 8, 128], BF16, 'sw_hT')
    pT = cx.ps([128, 8, 128], BF16, 'sw_pT')
    pA = [cx.ps([128, 512], F32, 'sw_pA%d' % i) for i in range(3)]
    pQT = cx.ps([128, 16, 128], BF16, 'sw_pQT')
    pS1 = cx.ps([128, 512], F32, 'sw_pS1')
    pO = cx.ps([128, 4, 65], F32, 'sw_pO')
    pS = [pA[2], pS1]
    qsq = cx.sb([128, 1024], F32, 'sw_qsq')
    qn1 = cx.sb([128, 1024], F32, 'sw_qn1')
    qn = cx.sb([128, 1024], BF16, 'sw_qn')
    ssq = sm('ssq', 16); rq = sm('rq', 16)
    ksq = cx.sb([128, 256], F32, 'sw_ksq')
    kn1 = cx.sb([128, 256], F32, 'sw_kn1')
    kn = cx.sb([128, 256], BF16, 'sw_kn')
    ssk = sm('ssk', 4); rk = sm('rk', 4)
    qT = cx.sb([64, 16, 128], BF16, 'sw_qT')
    kT = [cx.sb([64, 4, 128], BF16, 'sw_kT%d' % i) for i in range(2)]
    va = [cx.sb([128, 4, 65], BF16, 'sw_va%d' % i) for i in range(2)]
    for i in range(2):
        S.op('vector', lambda e, i=i: e.memset(va[i][:], 1.0), writes=K(va[i]))
    pe = [cx.sb([128, 512], BF16, 'sw_pe%d' % i) for i in range(2)]
    den = sm('den', 4); rden = sm('rden', 4)
    o = cx.sb([128, 1024], BF16, 'sw_o')
    oT = cx.sb([128, 8, 128], BF16, 'sw_oT')
    t1 = cx.sb([128, 1024], F32, 'sw_t1')
    xo = [cx.sb([128, 1024], F32, 'sw_xo%d' % i) for i in range(2)]

    def kv_part(xt, cur):
        emit_norm_mod(cx, nbuf, xt, mods['Am'], mods['Bm'], [h])
        emit_transposes(cx, h, 8, pT, hT, idb)
        for n in range(3):
            for k in range(8):
                S.op('tensor', lambda e, n=n, k=k: e.matmul(pA[n][:], lhsT=hT[:, k, :], rhs=wi[:, k, n * 512:(n + 1) * 512], start=(k == 0), stop=(k == 7)),
                     reads=K(hT, wi), writes=K(pA[n]))
        S.op('scalar', lambda e: e.activation(out=ksq[:], in_=pA[2][:, 0:256], func=AF.Square), reads=K(pA[2]), writes=K(ksq))
        S.op('vector', lambda e: e.tensor_reduce(out=ssk[:], in_=ksq[:].rearrange("p (h d) -> p h d", h=4), axis=AX.X, op=ALU.add), reads=K(ksq), writes=K(ssk))
        S.op('scalar', lambda e: e.activation(out=rk[:], in_=ssk[:], func=AF.Sqrt, bias=EPS, scale=1.0 / 64), reads=K(ssk), writes=K(rk))
        S.op('vector', lambda e: e.reciprocal(out=rk[:], in_=rk[:]), reads=K(rk), writes=K(rk))
        S.op('vector', lambda e: e.tensor_tensor(out=kn1[:].rearrange("p (h d) -> p h d", h=4), in0=pA[2][:, 0:256].rearrange("p (h d) -> p h d", h=4),
                                                in1=rk[:].unsqueeze(2).to_broadcast([128, 4, 64]), op=ALU.mult), reads=K(pA[2], rk), writes=K(kn1))
        S.op('gpsimd', lambda e: e.tensor_tensor(out=kn[:].rearrange("p (h d) -> p h d", h=4), in0=kn1[:].rearrange("p (h d) -> p h d", h=4),
                                                in1=gk[:].unsqueeze(1).to_broadcast([128, 4, 64]), op=ALU.mult), reads=K(kn1, gk), writes=K(kn))
        S.op('scalar', lambda e, cur=cur: e.copy(out=va[cur][:, :, 0:64], in_=pA[2][:, 256:512].rearrange("p (h d) -> p h d", h=4)), reads=K(pA[2]), writes=K(va[cur]))
        for j in range(4):
            S.op('tensor', lambda e, j=j: e.transpose(out=pT[0:64, j, :], in_=kn[:, j * 64:(j + 1) * 64], identity=idb[:]), reads=K(kn, idb), writes=K(pT))
        S.op('vector', lambda e, cur=cur: e.tensor_copy(out=kT[cur][:], in_=pT[0:64, 0:4, :]), reads=K(pT), writes=K(kT[cur]))

    xh = xts[1]
    S.dma('sync', lambda e: e.dma_start(out=xh[:], in_=x_halo), writes=K(xh))
    kv_part(xh, 1)

    for t in range(NT):
        cur = t % 2
        prv = 1 - cur
        xt = xts[cur]
        S.dma('sync', lambda e, t=t, xt=xt: e.dma_start(out=xt[:], in_=x_in[t * 128:(t + 1) * 128, :]), reads=[DK(x_in, t)], writes=K(xt))
        kv_part(xt, cur)
        for n in range(2):
            S.op('scalar', lambda e, n=n: e.activation(out=qsq[:, n * 512:(n + 1) * 512], in_=pA[n][:], func=AF.Square), reads=K(pA[n]), writes=K(qsq))
        S.op('vector', lambda e: e.tensor_reduce(out=ssq[:], in_=qsq[:].rearrange("p (h d) -> p h d", h=16), axis=AX.X, op=ALU.add), reads=K(qsq), writes=K(ssq))
        S.op('scalar', lambda e: e.activation(out=rq[:], in_=ssq[:], func=AF.Sqrt, bias=EPS, scale=1.0 / 64), reads=K(ssq), writes=K(rq))
        S.op('vector', lambda e: e.reciprocal(out=rq[:], in_=rq[:]), reads=K(rq), writes=K(rq))
        for n in range(2):
            S.op('vector', lambda e, n=n: e.tensor_tensor(out=qn1[:, n * 512:(n + 1) * 512].rearrange("p (h d) -> p h d", h=8), in0=pA[n][:].rearrange("p (h d) -> p h d", h=8),
                                                         in1=rq[:, n * 8:(n + 1) * 8].unsqueeze(2).to_broadcast([128, 8, 64]), op=ALU.mult), reads=K(pA[n], rq), writes=K(qn1))
        S.op('gpsimd', lambda e: e.tensor_tensor(out=qn[:].rearrange("p (h d) -> p h d", h=16), in0=qn1[:].rearrange("p (h d) -> p h d", h=16),
                                                in1=gq8[:].unsqueeze(1).to_broadcast([128, 16, 64]), op=ALU.mult), reads=K(qn1, gq8), writes=K(qn))
        for hh in range(16):
            S.op('tensor', lambda e, hh=hh: e.transpose(out=pQT[0:64, hh, :], in_=qn[:, hh * 64:(hh + 1) * 64], identity=idb[:]), reads=K(qn, idb), writes=K(pQT))
        S.op('scalar', lambda e: e.copy(out=qT[:], in_=pQT[0:64, :, :]), reads=K(pQT), writes=K(qT))
        mprev = mp0 if t == 0 else gtb
        for kv in range(4):
            rhs_q = qT[:, 4 * kv:4 * kv + 4, :]
            S.op('tensor', lambda e, kv=kv, rhs_q=rhs_q, prv=prv: e.matmul(pS[0][:], lhsT=kT[prv][:, kv, :], rhs=rhs_q, start=True, stop=True), reads=K(kT[prv], qT), writes=K(pS[0]))
            S.op('tensor', lambda e, kv=kv, rhs_q=rhs_q, cur=cur: e.matmul(pS[1][:], lhsT=kT[cur][:, kv, :], rhs=rhs_q, start=True, stop=True), reads=K(kT[cur], qT), writes=K(pS[1]))
            for i in range(2):
                S.op('scalar', lambda e, i=i: e.activation(out=pe[i][:], in_=pS[i][:], func=AF.Exp, bias=negc[:, 0:1], scale=1.0), reads=K(pS[i], negc), writes=K(pe[i]))
            S.op('vector', lambda e, mprev=mprev: e.tensor_tensor(out=pe[0][:].rearrange("p (h q) -> p h q", h=4), in0=pe[0][:].rearrange("p (h q) -> p h q", h=4),
                                                                 in1=mprev[:].unsqueeze(1).to_broadcast([128, 4, 128]), op=ALU.mult), reads=K(pe[0], mprev), writes=K(pe[0]))
            S.op('gpsimd', lambda e: e.tensor_tensor(out=pe[1][:].rearrange("p (h q) -> p h q", h=4), in0=pe[1][:].rearrange("p (h q) -> p h q", h=4),
                                                    in1=causb[:].unsqueeze(1).to_broadcast([128, 4, 128]), op=ALU.mult), reads=K(pe[1], causb), writes=K(pe[1]))
            for hh in range(4):
                S.op('tensor', lambda e, hh=hh, kv=kv, prv=prv: e.matmul(pO[:, hh, :], lhsT=pe[0][:, hh * 128:(hh + 1) * 128], rhs=va[prv][:, kv, :], start=True, stop=False),
                     reads=K(pe[0], va[prv]), writes=K(pO))
                S.op('tensor', lambda e, hh=hh, kv=kv, cur=cur: e.matmul(pO[:, hh, :], lhsT=pe[1][:, hh * 128:(hh + 1) * 128], rhs=va[cur][:, kv, :], start=False, stop=True),
                     reads=K(pe[1], va[cur]), writes=K(pO))
            S.op('vector', lambda e, kv=kv: e.tensor_tensor(out=den[:], in0=pO[:, :, 64], in1=esink[:, 4 * kv:4 * kv + 4], op=ALU.add), reads=K(pO, esink), writes=K(den))
            S.op('vector', lambda e: e.reciprocal(out=rden[:], in_=den[:]), reads=K(den), writes=K(rden))
            S.op('vector', lambda e, kv=kv: e.tensor_tensor(out=o[:, kv * 256:(kv + 1) * 256].rearrange("p (h d) -> p h d", h=4), in0=pO[:, :, 0:64],
                                                           in1=rden[:].unsqueeze(2).to_broadcast([128, 4, 64]), op=ALU.mult), reads=K(pO, rden), writes=K(o))
        emit_transposes(cx, o, 8, pT, oT, idb)
        for n in range(2):
            for k in range(8):
                S.op('tensor', lambda e, n=n, k=k: e.matmul(pA[n][:], lhsT=oT[:, k, :], rhs=wo[:, k, n * 512:(n + 1) * 512], start=(k == 0), stop=(k == 7)),
                     reads=K(oT, wo), writes=K(pA[n]))
        ob = xo[t % 2]
        for n in range(2):
            S.op('vector', lambda e, n=n: e.tensor_tensor(out=t1[:, n * 512:(n + 1) * 512], in0=pA[n][:], in1=mods['Gm'][:, n * 512:(n + 1) * 512], op=ALU.mult),
                 reads=K(pA[n], mods['Gm']), writes=K(t1))
        S.op('gpsimd', lambda e, ob=ob, xt=xt: e.tensor_tensor(out=ob[:], in0=t1[:], in1=xt[:], op=ALU.add), reads=K(t1, xt), writes=K(ob))
        S.dma('sync', lambda e, t=t, ob=ob: e.dma_start(out=x_out[t * 128:(t + 1) * 128, :], in_=ob[:]), reads=K(ob), writes=[DK(x_out, t)], is_output=True)


def emit_dn1(cx, x_in, NT, mods, w_in, conv_wT, a_log, dt_bias, x_halo, hasprev_d, oloc, RT, zs, st_out):
    S = cx.S
    idb = cx.consts['ident_bf']
    idf = cx.consts['ident']
    causf = cx.consts['causT']
    lsf = cx.consts['lstrict']
    onf = cx.consts['ones']

    def op(eng, fn, r, w):
        S.op(eng, fn, reads=K(*r), writes=K(*w))

    wi = cx.sb([128, 8, 3088], BF16, 'dn_wi')
    for k in range(8):
        S.dma('gpsimd', lambda e, k=k: e.dma_start(out=wi[:, k, :], in_=w_in[k * 128:(k + 1) * 128, :]), writes=K(wi))
    cw = cx.sb([128, 16, 4], F32, 'dn_cw')
    S.dma('sync', lambda e: e.dma_start(out=cw[:], in_=conv_wT.rearrange("(c p) j -> p c j", p=128)), writes=K(cw))
    Aexp = cx.sb([128, 8], F32, 'dn_A')
    dtb = cx.sb([128, 8], F32, 'dn_dtb')
    hasp = cx.sb([128, 1], F32, 'dn_hasp')
    S.dma('sync', lambda e: e.dma_start(out=Aexp[:], in_=a_log.partition_broadcast(128)), writes=K(Aexp))
    S.dma('sync', lambda e: e.dma_start(out=dtb[:], in_=dt_bias.partition_broadcast(128)), writes=K(dtb))
    S.dma('sync', lambda e: e.dma_start(out=hasp[:], in_=hasprev_d), writes=K(hasp))
    op('scalar', lambda e: e.activation(out=Aexp[:], in_=Aexp[:], func=AF.Exp), [Aexp], [Aexp])
    op('vector', lambda e: e.tensor_scalar(out=Aexp[:], in0=Aexp[:], scalar1=-1.0, scalar2=None, op0=ALU.mult), [Aexp], [Aexp])

    F = [cx.ps([128, 512], F32, 'dn_F%d' % i) for i in range(7)]
    BT = cx.ps([128, 8, 128], BF16, 'dn_BT')

    def fv(i, a):
        return F[i][:].rearrange("p (a b) -> p a b", a=a)

    nbuf = NormBufs(cx, 'dn')
    xt = cx.sb([128, 1024], F32, 'dn_x')
    h = cx.sb([128, 1024], BF16, 'dn_h')
    hT = cx.sb([128, 8, 128], BF16, 'dn_hT')
    XC = cx.sb([128, 16, 131], F32, 'dn_XC')
    carry = cx.sb([128, 16, 3], F32, 'dn_carry')
    qkv = cx.sb([128, 16, 128], F32, 'dn_qkv')
    vTb = cx.sb([128, 8, 128], BF16, 'dn_vTb')
    sq = cx.sb([128, 8, 128], F32, 'dn_sq')
    rn = cx.sb([128, 8, 128], F32, 'dn_rn')
    qTn = cx.sb([128, 4, 128], BF16, 'dn_qTn')
    kTn = cx.sb([128, 4, 128], BF16, 'dn_kTn')
    Ktok = cx.sb([128, 4, 128], BF16, 'dn_Ktok')
    Vtok = cx.sb([128, 8, 128], BF16, 'dn_Vtok')
    zsil = cx.sb([128, 1024], BF16, 'dn_zsil')
    gt = cx.sb([128, 16], F32, 'dn_gt')
    ctmp = cx.sb([128, 128], F32, 'dn_ctmp')

    def sm(name, w=8):
        return cx.sb([128, w], F32, 'dn_' + name)
    beta = sm('beta'); nbeta = sm('nbeta'); xa = sm('xa'); ax = sm('ax'); ee = sm('ee'); ll = sm('ll'); sp = sm('sp'); g = sm('g')
    gc = sm('gc'); gtot = sm('gtot'); ngc = sm('ngc'); egc = sm('egc'); bg = sm('bg'); kdsc = sm('kdsc'); gl = sm('gl')
    Grep = cx.sb([128, 8, 128], F32, 'dn_Grep')
    dif = cx.sb([128, 8, 128], F32, 'dn_dif')
    DmT = cx.sb([128, 8, 128], F32, 'dn_DmT')
    DmTs = cx.sb([128, 8, 128], F32, 'dn_DmTs')
    egrow = cx.sb([128, 8, 128], F32, 'dn_egrow')
    Bm = cx.sb([128, 8, 128], F32, 'dn_Bm')
    qkT = cx.sb([128, 8, 128], BF16, 'dn_qkT')
    M = cx.sb([128, 8, 128], F32, 'dn_M')
    MT = cx.sb([128, 8, 128], F32, 'dn_MT')
    P = cx.sb([128, 8, 128], F32, 'dn_P')
    Yb = cx.sb([128, 8, 128], BF16, 'dn_Yb')
    u = cx.sb([128, 8, 128], F32, 'dn_u')
    wT = cx.sb([128, 8, 128], BF16, 'dn_wT')
    qdT = cx.sb([128, 8, 128], BF16, 'dn_qdT')
    kd = cx.sb([128, 8, 128], BF16, 'dn_kd')
    Kbg = cx.sb([128, 8, 128], BF16, 'dn_Kbg')
    Vb = cx.sb([128, 8, 128], BF16, 'dn_Vb')
    SA = cx.sb([128, 8, 256], F32, 'dn_SA')
    SAb = cx.sb([128, 8, 256], BF16, 'dn_SAb')
    vnew = cx.sb([128, 8, 256], BF16, 'dn_vnew')
    Ot = cx.sb([128, 1024], F32, 'dn_Ot')
    RTt = cx.sb([128, 8, 128], BF16, 'dn_RTt')

    op('vector', lambda e: e.memset(SA[:], 0.0), [], [SA])
    op('vector', lambda e: e.tensor_copy(out=SA[:, :, 128:256], in_=idf[:].unsqueeze(1).to_broadcast([128, 8, 128])), [idf, SA], [SA])
    op('vector', lambda e: e.tensor_copy(out=SAb[:], in_=SA[:]), [SA], [SAb])

    def proj_T(ncols, col0):
        for c in range(16):
            bank = c // 4
            for k in range(8):
                S.op('tensor', lambda e, c=c, k=k, bank=bank: e.matmul(fv(bank, 4)[:, c % 4, 0:ncols], lhsT=wi[:, k, c * 128:(c + 1) * 128], rhs=hT[:, k, col0:col0 + ncols],
                                                                      start=(k == 0), stop=(k == 7)), reads=K(hT, wi), writes=K(F[bank]))

    S.dma('sync', lambda e: e.dma_start(out=xt[:], in_=x_halo), writes=K(xt))
    emit_norm_mod(cx, nbuf, xt, mods['Am'], mods['Bm'], [h])
    emit_transposes(cx, h, 8, BT, hT, idb)
    proj_T(4, 124)
    for bank in range(4):
        op('vector', lambda e, bank=bank: e.tensor_scalar(out=carry[:, bank * 4:(bank + 1) * 4, :], in0=fv(bank, 4)[:, :, 1:4], scalar1=hasp[:, 0:1], scalar2=None, op0=ALU.mult),
           [F[bank], hasp], [carry])

    for t in range(NT):
        S.dma('sync', lambda e, t=t: e.dma_start(out=xt[:], in_=x_in[t * 128:(t + 1) * 128, :]), reads=[DK(x_in, t)], writes=K(xt))
        emit_norm_mod(cx, nbuf, xt, mods['Am'], mods['Bm'], [h])
        emit_transposes(cx, h, 8, BT, hT, idb)
        proj_T(128, 0)
        op('vector', lambda e: e.tensor_copy(out=XC[:, :, 0:3], in_=carry[:]), [carry, XC], [XC])
        for bank in range(4):
            op('scalar', lambda e, bank=bank: e.copy(out=XC[:, bank * 4:(bank + 1) * 4, 3:131], in_=fv(bank, 4)), [F[bank], XC], [XC])
        op('vector', lambda e: e.tensor_copy(out=carry[:], in_=XC[:, :, 128:131]), [XC], [carry])
        for n in range(2):
            for k in range(8):
                S.op('tensor', lambda e, n=n, k=k: e.matmul(F[4 + n][:], lhsT=hT[:, k, :], rhs=wi[:, k, 2048 + n * 512:2048 + (n + 1) * 512], start=(k == 0), stop=(k == 7)),
                     reads=K(hT, wi), writes=K(F[4 + n]))
        for k in range(8):
            S.op('tensor', lambda e, k=k: e.matmul(F[6][:, 0:16], lhsT=hT[:, k, :], rhs=wi[:, k, 3072:3088], start=(k == 0), stop=(k == 7)), reads=K(hT, wi), writes=K(F[6]))
        for n in range(2):
            op('scalar', lambda e, n=n: e.activation(out=zsil[:, n * 512:(n + 1) * 512], in_=F[4 + n][:], func=AF.Silu), [F[4 + n]], [zsil])
        S.dma('sync', lambda e, t=t: e.dma_start(out=zs[t * 128:(t + 1) * 128, :], in_=zsil[:]), reads=K(zsil), writes=[DK(zs, t)], is_output=True)
        op('vector', lambda e: e.tensor_copy(out=gt[:], in_=F[6][:, 0:16]), [F[6]], [gt])
        if DN_PHASE <= 1:
            continue
        op('scalar', lambda e: e.activation(out=beta[:], in_=gt[:, 0:8], func=AF.Sigmoid), [gt], [beta])
        op('vector', lambda e: e.tensor_scalar(out=nbeta[:], in0=beta[:], scalar1=-1.0, scalar2=None, op0=ALU.mult), [beta], [nbeta])
        op('vector', lambda e: e.tensor_tensor(out=xa[:], in0=gt[:, 8:16], in1=dtb[:], op=ALU.add), [gt, dtb], [xa])
        op('scalar', lambda e: e.activation(out=ax[:], in_=xa[:], func=AF.Abs), [xa], [ax])
        op('scalar', lambda e: e.activation(out=ee[:], in_=ax[:], func=AF.Exp, scale=-1.0), [ax], [ee])
        op('scalar', lambda e: e.activation(out=ll[:], in_=ee[:], func=AF.Ln, bias=1.0, scale=1.0), [ee], [ll])
        op('vector', lambda e: e.scalar_tensor_tensor(out=sp[:], in0=xa[:], scalar=0.0, in1=ll[:], op0=ALU.max, op1=ALU.add), [xa, ll], [sp])
        op('vector', lambda e: e.tensor_tensor(out=g[:], in0=sp[:], in1=Aexp[:], op=ALU.mult), [sp, Aexp], [g])
        S.op('tensor', lambda e: e.matmul(F[6][:, 16:24], lhsT=causf[:], rhs=g[:], start=True, stop=True), reads=K(causf, g), writes=K(F[6]))
        S.op('tensor', lambda e: e.matmul(F[6][:, 24:32], lhsT=onf[:], rhs=g[:], start=True, stop=True), reads=K(onf, g), writes=K(F[6]))
        op('vector', lambda e: e.tensor_copy(out=gc[:], in_=F[6][:, 16:24]), [F[6]], [gc])
        op('vector', lambda e: e.tensor_copy(out=gtot[:], in_=F[6][:, 24:32]), [F[6]], [gtot])
        op('vector', lambda e: e.tensor_scalar(out=ngc[:], in0=gc[:], scalar1=-1.0, scalar2=None, op0=ALU.mult), [gc], [ngc])
        op('scalar', lambda e: e.activation(out=egc[:], in_=gc[:], func=AF.Exp), [gc], [egc])
        op('vector', lambda e: e.tensor_tensor(out=bg[:], in0=egc[:], in1=beta[:], op=ALU.mult), [egc, beta], [bg])
        op('vector', lambda e: e.tensor_tensor(out=kdsc[:], in0=gtot[:], in1=gc[:], op=ALU.subtract), [gtot, gc], [kdsc])
        op('scalar', lambda e: e.activation(out=kdsc[:], in_=kdsc[:], func=AF.Exp), [kdsc], [kdsc])
        op('scalar', lambda e: e.activation(out=gl[:], in_=gtot[:], func=AF.Exp), [gtot], [gl])
        op('vector', lambda e: e.tensor_copy(out=Grep[:], in_=g[:].unsqueeze(2).to_broadcast([128, 8, 128])), [g], [Grep])
        for hh in range(8):
            bank = 4 + hh // 4
            S.op('tensor', lambda e, hh=hh, bank=bank: e.matmul(fv(bank, 4)[:, hh % 4, :], lhsT=Grep[:, hh, :], rhs=causf[:], start=True, stop=True),
                 reads=K(Grep, causf), writes=K(F[bank]))
        for hh in range(8):
            bank = 4 + hh // 4
            op('vector', lambda e, hh=hh, bank=bank: e.tensor_scalar(out=dif[:, hh, :], in0=fv(bank, 4)[:, hh % 4, :], scalar1=gc[:, hh:hh + 1], scalar2=0.0, op0=ALU.subtract, op1=ALU.min),
               [F[bank], gc], [dif])
        op('scalar', lambda e: e.activation(out=DmT[:], in_=dif[:], func=AF.Exp), [dif], [DmT])
        for half in range(2):
            op('scalar', lambda e, half=half: e.activation(out=egrow[:, half * 4:(half + 1) * 4, :], in_=fv(4 + half, 4), func=AF.Exp), [F[4 + half]], [egrow])
        op('gpsimd', lambda e: e.tensor_tensor(out=DmTs[:], in0=DmT[:], in1=lsf[:].unsqueeze(1).to_broadcast([128, 8, 128]), op=ALU.mult), [DmT, lsf], [DmTs])
        op('vector', lambda e: e.tensor_tensor(out=DmT[:], in0=DmT[:], in1=causf[:].unsqueeze(1).to_broadcast([128, 8, 128]), op=ALU.mult), [DmT, causf], [DmT])
        if DN_PHASE <= 2:
            continue
        for c in range(16):
            eng = 'vector' if c % 2 == 0 else 'gpsimd'
            op(eng, lambda e, c=c: e.tensor_scalar(out=qkv[:, c, :], in0=XC[:, c, 0:128], scalar1=cw[:, c, 0:1], scalar2=None, op0=ALU.mult), [XC, cw], [(qkv.name, c)])
            for j in range(1, 4):
                if eng == 'vector':
                    op(eng, lambda e, c=c, j=j: e.scalar_tensor_tensor(out=qkv[:, c, :], in0=XC[:, c, j:j + 128], scalar=cw[:, c, j:j + 1], in1=qkv[:, c, :], op0=ALU.mult, op1=ALU.add),
                       [XC, cw, (qkv.name, c)], [(qkv.name, c)])
                else:
                    op(eng, lambda e, c=c, j=j: e.tensor_scalar(out=ctmp[:], in0=XC[:, c, j:j + 128], scalar1=cw[:, c, j:j + 1], scalar2=None, op0=ALU.mult), [XC, cw], [ctmp])
                    op(eng, lambda e, c=c: e.tensor_tensor(out=qkv[:, c, :], in0=qkv[:, c, :], in1=ctmp[:], op=ALU.add), [ctmp, (qkv.name, c)], [(qkv.name, c)])
        qkeys = [(qkv.name, c) for c in range(16)]
        op('scalar', lambda e: e.activation(out=qkv[:, 0:8, :], in_=qkv[:, 0:8, :], func=AF.Silu), qkeys[0:8], qkeys[0:8])
        op('scalar', lambda e: e.activation(out=vTb[:], in_=qkv[:, 8:16, :], func=AF.Silu), qkeys[8:16], [vTb])
        op('vector', lambda e: e.tensor_tensor(out=sq[:], in0=qkv[:, 0:8, :], in1=qkv[:, 0:8, :], op=ALU.mult), qkeys[0:8], [sq])
        for c in range(8):
            bank = c // 4
            S.op('tensor', lambda e, c=c, bank=bank: e.matmul(fv(bank, 4)[:, c % 4, :], lhsT=onf[:], rhs=sq[:, c, :], start=True, stop=True), reads=K(onf, sq), writes=K(F[bank]))
        for bank in range(2):
            op('scalar', lambda e, bank=bank: e.activation(out=rn[:, bank * 4:(bank + 1) * 4, :], in_=fv(bank, 4), func=AF.Sqrt, bias=EPS, scale=1.0), [F[bank]], [rn])
        op('vector', lambda e: e.reciprocal(out=rn[:], in_=rn[:]), [rn], [rn])
        op('vector', lambda e: e.scalar_tensor_tensor(out=qTn[:], in0=qkv[:, 0:4, :], scalar=128.0 ** -0.5, in1=rn[:, 0:4, :], op0=ALU.mult, op1=ALU.mult), qkeys[0:4] + [rn], [qTn])
        op('gpsimd', lambda e: e.tensor_tensor(out=kTn[:], in0=qkv[:, 4:8, :], in1=rn[:, 4:8, :], op=ALU.mult), qkeys[4:8] + [rn], [kTn])
        if DN_PHASE <= 3:
            continue
        for j in range(4):
            S.op('tensor', lambda e, j=j: e.transpose(out=BT[:, j, :], in_=kTn[:, j, :], identity=idb[:]), reads=K(kTn, idb), writes=K(BT))
        op('scalar', lambda e: e.copy(out=Ktok[:], in_=BT[:, 0:4, :]), [BT], [Ktok])
        for j in range(8):
            S.op('tensor', lambda e, j=j: e.transpose(out=BT[:, j, :], in_=vTb[:, j, :], identity=idb[:]), reads=K(vTb, idb), writes=K(BT))
        op('vector', lambda e: e.tensor_copy(out=Vtok[:], in_=BT[:]), [BT], [Vtok])
        op('vector', lambda e: e.tensor_tensor(out=Vb[:], in0=Vtok[:], in1=beta[:].unsqueeze(2).to_broadcast([128, 8, 128]), op=ALU.mult), [Vtok, beta], [Vb])
        for j in range(4):
            kb = Ktok[:, j, :].unsqueeze(1).to_broadcast([128, 2, 128])
            op('vector', lambda e, j=j, kb=kb: e.tensor_tensor(out=Kbg[:, 2 * j:2 * j + 2, :], in0=kb, in1=bg[:, 2 * j:2 * j + 2].unsqueeze(2).to_broadcast([128, 2, 128]), op=ALU.mult),
               [Ktok, bg], [Kbg])
            op('vector', lambda e, j=j, kb=kb: e.tensor_tensor(out=kd[:, 2 * j:2 * j + 2, :], in0=kb, in1=kdsc[:, 2 * j:2 * j + 2].unsqueeze(2).to_broadcast([128, 2, 128]), op=ALU.mult),
               [Ktok, kdsc], [kd])
            qb = qTn[:, j, :].unsqueeze(1).to_broadcast([128, 2, 128])
            op('vector', lambda e, j=j, qb=qb: e.tensor_tensor(out=qdT[:, 2 * j:2 * j + 2, :], in0=qb, in1=egrow[:, 2 * j:2 * j + 2, :], op=ALU.mult), [qTn, egrow], [qdT])
        if DN_PHASE <= 4:
            continue
        for j in range(4):
            S.op('tensor', lambda e, j=j: e.matmul(fv(0, 4)[:, j, :], lhsT=kTn[:, j, :], rhs=kTn[:, j, :], start=True, stop=True), reads=K(kTn), writes=K(F[0]))
            S.op('tensor', lambda e, j=j: e.matmul(fv(1, 4)[:, j, :], lhsT=kTn[:, j, :], rhs=qTn[:, j, :], start=True, stop=True), reads=K(kTn, qTn), writes=K(F[1]))
        if DN_PHASE <= 4.1:
            continue
        for hh in range(8):
            j = hh // 2
            op('vector', lambda e, j=j, hh=hh: e.tensor_tensor(out=Bm[:, hh, :], in0=fv(0, 4)[:, j, :], in1=DmTs[:, hh, :], op=ALU.mult), [F[0], DmTs], [Bm])
            op('vector', lambda e, j=j, hh=hh: e.tensor_tensor(out=qkT[:, hh, :], in0=fv(1, 4)[:, j, :], in1=DmT[:, hh, :], op=ALU.mult), [F[1], DmT], [qkT])
        if DN_PHASE <= 4.25:
            continue
        for hh in range(8):
            bank = 2 + hh // 4
            S.op('tensor', lambda e, hh=hh, bank=bank: e.transpose(out=fv(bank, 4)[:, hh % 4, :], in_=Bm[:, hh, :], identity=idf[:]), reads=K(Bm, idf), writes=K(F[bank]))
        for hh in range(8):
            bank = 2 + hh // 4
            op('vector', lambda e, hh=hh, bank=bank: e.tensor_scalar(out=M[:, hh, :], in0=fv(bank, 4)[:, hh % 4, :], scalar1=nbeta[:, hh:hh + 1], scalar2=None, op0=ALU.mult),
               [F[bank], nbeta], [M])
        if DN_PHASE <= 4.5:
            continue
        for hh in range(8):
            bank = 4 + hh // 4
            S.op('tensor', lambda e, hh=hh, bank=bank: e.transpose(out=fv(bank, 4)[:, hh % 4, :], in_=M[:, hh, :], identity=idf[:]), reads=K(M, idf), writes=K(F[bank]))
        if DN_PHASE <= 4.6:
            continue
        for half in range(2):
            op('scalar', lambda e, half=half: e.copy(out=MT[:, half * 4:(half + 1) * 4, :], in_=fv(4 + half, 4)), [F[4 + half]], [MT])
            if DN_PHASE <= 4.7:
                continue
            op('vector', lambda e, half=half: e.tensor_tensor(out=P[:, half * 4:(half + 1) * 4, :], in0=MT[:, half * 4:(half + 1) * 4, :], in1=idf[:].unsqueeze(1).to_broadcast([128, 4, 128]), op=ALU.add),
               [MT, idf], [P])
        if DN_PHASE <= 5:
            continue
        for lev in range(6):
            for hh in range(8):
                bank = hh // 4
                S.op('tensor', lambda e, hh=hh, bank=bank: e.matmul(fv(bank, 4)[:, hh % 4, :], lhsT=MT[:, hh, :], rhs=M[:, hh, :], start=True, stop=True), reads=K(MT, M), writes=K(F[bank]))
            if lev < 5:
                for hh in range(8):
                    bank = 2 + hh // 4
                    S.op('tensor', lambda e, hh=hh, bank=bank: e.matmul(fv(bank, 4)[:, hh % 4, :], lhsT=M[:, hh, :], rhs=MT[:, hh, :], start=True, stop=True), reads=K(MT, M), writes=K(F[bank]))
            for half in range(2):
                op('scalar', lambda e, half=half: e.copy(out=M[:, half * 4:(half + 1) * 4, :], in_=fv(half, 4)), [F[half], M], [M])
            if lev < 5:
                for half in range(2):
                    op('vector', lambda e, half=half: e.tensor_copy(out=MT[:, half * 4:(half + 1) * 4, :], in_=fv(2 + half, 4)), [F[2 + half], MT], [MT])
            for hh in range(8):
                bank = 4 + hh // 4
                S.op('tensor', lambda e, hh=hh, bank=bank: e.matmul(fv(bank, 4)[:, hh % 4, :], lhsT=M[:, hh, :], rhs=P[:, hh, :], start=True, stop=True), reads=K(M, P), writes=K(F[bank]))
            for half in range(2):
                op('vector', lambda e, half=half: e.tensor_tensor(out=P[:, half * 4:(half + 1) * 4, :], in0=fv(4 + half, 4), in1=P[:, half * 4:(half + 1) * 4, :], op=ALU.add),
                   [F[4 + half], P], [P])
        op('scalar', lambda e: e.copy(out=Yb[:], in_=P[:]), [P], [Yb])
        for hh in range(8):
            S.op('tensor', lambda e, hh=hh: e.matmul(fv(hh // 4, 4)[:, hh % 4, :], lhsT=Yb[:, hh, :], rhs=Vb[:, hh, :], start=True, stop=True), reads=K(Yb, Vb), writes=K(F[hh // 4]))
            S.op('tensor', lambda e, hh=hh: e.matmul(fv(2 + hh // 4, 4)[:, hh % 4, :], lhsT=Kbg[:, hh, :], rhs=Yb[:, hh, :], start=True, stop=True), reads=K(Yb, Kbg), writes=K(F[2 + hh // 4]))
        for half in range(2):
            op('vector', lambda e, half=half: e.tensor_copy(out=u[:, half * 4:(half + 1) * 4, :], in_=fv(half, 4)), [F[half]], [u])
            op('scalar', lambda e, half=half: e.copy(out=wT[:, half * 4:(half + 1) * 4, :], in_=fv(2 + half, 4)), [F[2 + half]], [wT])
        if DN_PHASE <= 6:
            continue
        for grp in range(2):
            hs_ = range(grp * 4, grp * 4 + 4)
            for hh in hs_:
                bank = (hh % 4) // 2
                S.op('tensor', lambda e, hh=hh, bank=bank: e.matmul(fv(bank, 2)[:, hh % 2, :], lhsT=wT[:, hh, :], rhs=SAb[:, hh, :], start=True, stop=True), reads=K(wT, SAb), writes=K(F[bank]))
            for hh in hs_:
                bank = (hh % 4) // 2
                op('vector', lambda e, hh=hh, bank=bank: e.tensor_tensor(out=vnew[:, hh, 0:128], in0=u[:, hh, :], in1=fv(bank, 2)[:, hh % 2, 0:128], op=ALU.subtract), [u, F[bank]], [vnew])
                op('scalar', lambda e, hh=hh, bank=bank: e.activation(out=vnew[:, hh, 128:256], in_=fv(bank, 2)[:, hh % 2, 128:256], func=AF.Copy, scale=-1.0), [F[bank]], [vnew])
            for hh in hs_:
                i4 = hh % 4
                S.op('tensor', lambda e, hh=hh, i4=i4: e.matmul(fv(2, 4)[:, i4, :], lhsT=qdT[:, hh, :], rhs=SAb[:, hh, 0:128], start=True, stop=False), reads=K(qdT, SAb), writes=K(F[2]))
                S.op('tensor', lambda e, hh=hh, i4=i4: e.matmul(fv(2, 4)[:, i4, :], lhsT=qkT[:, hh, :], rhs=vnew[:, hh, 0:128], start=False, stop=True), reads=K(qkT, vnew), writes=K(F[2]))
                S.op('tensor', lambda e, hh=hh, i4=i4: e.matmul(fv(3, 4)[:, i4, :], lhsT=SAb[:, hh, 128:256], rhs=qdT[:, hh, :], start=True, stop=False), reads=K(qdT, SAb), writes=K(F[3]))
                S.op('tensor', lambda e, hh=hh, i4=i4: e.matmul(fv(3, 4)[:, i4, :], lhsT=vnew[:, hh, 128:256], rhs=qkT[:, hh, :], start=False, stop=True), reads=K(qkT, vnew), writes=K(F[3]))
            for hh in hs_:
                bank = 4 + (hh % 4) // 2
                S.op('tensor', lambda e, hh=hh, bank=bank: e.matmul(fv(bank, 2)[:, hh % 2, :], lhsT=kd[:, hh, :], rhs=vnew[:, hh, :], start=True, stop=True), reads=K(kd, vnew), writes=K(F[bank]))
            op('scalar', lambda e, grp=grp: e.copy(out=Ot[:, grp * 512:(grp + 1) * 512], in_=F[2][:]), [F[2]], [Ot])
            op('vector', lambda e, grp=grp: e.tensor_copy(out=RTt[:, grp * 4:(grp + 1) * 4, :], in_=fv(3, 4)), [F[3]], [RTt])
            for hh in hs_:
                bank = 4 + (hh % 4) // 2
                op('vector', lambda e, hh=hh, bank=bank: e.scalar_tensor_tensor(out=SA[:, hh, :], in0=SA[:, hh, :], scalar=gl[:, hh:hh + 1], in1=fv(bank, 2)[:, hh % 2, :], op0=ALU.mult, op1=ALU.add),
                   [SA, gl, F[bank]], [SA])
            op('scalar', lambda e, grp=grp: e.copy(out=SAb[:, grp * 4:(grp + 1) * 4, :], in_=SA[:, grp * 4:(grp + 1) * 4, :]), [SA, SAb], [SAb])
        S.dma('sync', lambda e, t=t: e.dma_start(out=oloc[t * 128:(t + 1) * 128, :], in_=Ot[:]), reads=K(Ot), writes=[DK(oloc, t)], is_output=True)
        S.dma('sync', lambda e, t=t: e.dma_start(out=RT[:, :, t * 128:(t + 1) * 128].rearrange("h d c -> d h c"), in_=RTt[:]), reads=K(RTt), writes=[DK(RT, t)], is_output=True)
    S.dma('sync', lambda e: e.dma_start(out=st_out.rearrange("h d n -> d h n"), in_=SA[:]), reads=K(SA), writes=[DK(st_out)], is_output=True)


def emit_dn2(cx, x_in, x_out, NT, mods, prevPT, prevS, nprev, oloc, RT, zs, o_norm, w_out):
    S = cx.S
    idb = cx.consts['ident_bf']

    def op(eng, fn, r, w):
        S.op(eng, fn, reads=K(*r), writes=K(*w))
    wo = cx.sb([128, 8, 1024], BF16, 'd2_wo')
    for k in range(8):
        S.dma('gpsimd', lambda e, k=k: e.dma_start(out=wo[:, k, :], in_=w_out[k * 128:(k + 1) * 128, :]), writes=K(wo))
    og = cx.sb([128, 128], F32, 'd2_og')
    S.dma('sync', lambda e: e.dma_start(out=og[:], in_=o_norm.partition_broadcast(128)), writes=K(og))
    pC = [cx.ps([128, 512], F32, 'd2_pC%d' % i) for i in range(2)]
    pY = [cx.ps([128, 512], F32, 'd2_pY%d' % i) for i in range(2)]
    pT = cx.ps([128, 8, 128], BF16, 'd2_pT')

    def cv(i):
        return pC[i][:].rearrange("p (a b) -> p a b", a=4)
    Sst = cx.sb([128, 8, 128], F32, 'd2_S')
    op('vector', lambda e: e.memset(Sst[:], 0.0), [], [Sst])
    PTj = cx.sb([128, 8, 128], F32, 'd2_PTj')
    Slj = cx.sb([128, 8, 128], F32, 'd2_Slj')
    for j in range(nprev):
        S.dma('sync', lambda e, j=j: e.dma_start(out=PTj[:], in_=prevPT[j].rearrange("h a b -> a h b")), writes=K(PTj))
        S.dma('sync', lambda e, j=j: e.dma_start(out=Slj[:], in_=prevS[j].rearrange("h a b -> a h b")), writes=K(Slj))
        for hh in range(8):
            S.op('tensor', lambda e, hh=hh: e.matmul(cv(hh // 4)[:, hh % 4, :], lhsT=PTj[:, hh, :], rhs=Sst[:, hh, :], start=True, stop=True), reads=K(PTj, Sst), writes=K(pC[hh // 4]))
        for half in range(2):
            op('vector', lambda e, half=half: e.tensor_tensor(out=Sst[:, half * 4:(half + 1) * 4, :], in0=cv(half), in1=Slj[:, half * 4:(half + 1) * 4, :], op=ALU.add),
               [pC[half], Slj, Sst], [Sst])
    Sb = cx.sb([128, 8, 128], BF16, 'd2_Sb')
    op('vector', lambda e: e.tensor_copy(out=Sb[:], in_=Sst[:]), [Sst], [Sb])

    xts = [cx.sb([128, 1024], F32, 'd2_x%d' % i) for i in range(2)]
    ol = [cx.sb([128, 1024], F32, 'd2_ol%d' % i) for i in range(2)]
    rt = [cx.sb([128, 8, 128], BF16, 'd2_rt%d' % i) for i in range(2)]
    zt = [cx.sb([128, 1024], BF16, 'd2_zt%d' % i) for i in range(2)]
    o = cx.sb([128, 1024], F32, 'd2_o')
    osq = cx.sb([128, 1024], F32, 'd2_osq')
    ss = cx.sb([128, 8], F32, 'd2_ss')
    rs = cx.sb([128, 8], F32, 'd2_rs')
    on = cx.sb([128, 1024], F32, 'd2_on')
    ob = cx.sb([128, 1024], BF16, 'd2_ob')
    oT = cx.sb([128, 8, 128], BF16, 'd2_oT')
    t1 = cx.sb([128, 1024], F32, 'd2_t1')
    xo = [cx.sb([128, 1024], F32, 'd2_xo%d' % i) for i in range(2)]
    for t in range(NT):
        b = t % 2
        S.dma('sync', lambda e, t=t, b=b: e.dma_start(out=xts[b][:], in_=x_in[t * 128:(t + 1) * 128, :]), reads=[DK(x_in, t)], writes=K(xts[b]))
        S.dma('sync', lambda e, t=t, b=b: e.dma_start(out=ol[b][:], in_=oloc[t * 128:(t + 1) * 128, :]), reads=[DK(oloc, t)], writes=K(ol[b]))
        S.dma('sync', lambda e, t=t, b=b: e.dma_start(out=rt[b][:], in_=RT[:, :, t * 128:(t + 1) * 128].rearrange("h d c -> d h c")), reads=[DK(RT, t)], writes=K(rt[b]))
        S.dma('sync', lambda e, t=t, b=b: e.dma_start(out=zt[b][:], in_=zs[t * 128:(t + 1) * 128, :]), reads=[DK(zs, t)], writes=K(zt[b]))
        for hh in range(8):
            S.op('tensor', lambda e, hh=hh, b=b: e.matmul(cv(hh // 4)[:, hh % 4, :], lhsT=rt[b][:, hh, :], rhs=Sb[:, hh, :], start=True, stop=True), reads=K(rt[b], Sb), writes=K(pC[hh // 4]))
        for half in range(2):
            op('vector', lambda e, half=half, b=b: e.tensor_tensor(out=o[:, half * 512:(half + 1) * 512], in0=pC[half][:], in1=ol[b][:, half * 512:(half + 1) * 512], op=ALU.add),
               [pC[half], ol[b]], [o])
        op('scalar', lambda e: e.activation(out=osq[:], in_=o[:], func=AF.Square), [o], [osq])
        op('vector', lambda e: e.tensor_reduce(out=ss[:], in_=osq[:].rearrange("p (h d) -> p h d", h=8), axis=AX.X, op=ALU.add), [osq], [ss])
        op('scalar', lambda e: e.activation(out=rs[:], in_=ss[:], func=AF.Sqrt, bias=EPS, scale=1.0 / 128), [ss], [rs])
        op('vector', lambda e: e.reciprocal(out=rs[:], in_=rs[:]), [rs], [rs])
        op('vector', lambda e: e.tensor_tensor(out=on[:].rearrange("p (h d) -> p h d", h=8), in0=o[:].rearrange("p (h d) -> p h d", h=8),
                                              in1=rs[:].unsqueeze(2).to_broadcast([128, 8, 128]), op=ALU.mult), [o, rs], [on])
        op('gpsimd', lambda e: e.tensor_tensor(out=on[:].rearrange("p (h d) -> p h d", h=8), in0=on[:].rearrange("p (h d) -> p h d", h=8),
                                              in1=og[:].unsqueeze(1).to_broadcast([128, 8, 128]), op=ALU.mult), [on, og], [on])
        op('vector', lambda e, b=b: e.tensor_tensor(out=ob[:], in0=on[:], in1=zt[b][:], op=ALU.mult), [on, zt[b]], [ob])
        emit_transposes(cx, ob, 8, pT, oT, idb)
        for n in range(2):
            for k in range(8):
                S.op('tensor', lambda e, n=n, k=k: e.matmul(pY[n][:], lhsT=oT[:, k, :], rhs=wo[:, k, n * 512:(n + 1) * 512], start=(k == 0), stop=(k == 7)),
                     reads=K(oT, wo), writes=K(pY[n]))
        for n in range(2):
            op('vector', lambda e, n=n: e.tensor_tensor(out=t1[:, n * 512:(n + 1) * 512], in0=pY[n][:], in1=mods['Gm'][:, n * 512:(n + 1) * 512], op=ALU.mult), [pY[n], mods['Gm']], [t1])
        op('gpsimd', lambda e, b=b: e.tensor_tensor(out=xo[b][:], in0=t1[:], in1=xts[b][:], op=ALU.add), [t1, xts[b]], [xo[b]])
        S.dma('sync', lambda e, t=t, b=b: e.dma_start(out=x_out[t * 128:(t + 1) * 128, :], in_=xo[b][:]), reads=K(xo[b]), writes=[DK(x_out, t)], is_output=True)


NT_FULL = 32
TPC = NT_FULL * 128


class Prog:
    def __init__(self):
        self.nc = bass.Bass("TRN2", target_bir_lowering=False)
        self.cx = Ctx(self.nc)

    def din(self, name, shape, dt=F32):
        return self.nc.dram_tensor(name, list(shape), dt, kind="ExternalInput").ap()

    def dout(self, name, shape, dt=F32):
        return self.nc.dram_tensor(name, list(shape), dt, kind="ExternalOutput").ap()

    def dint(self, name, shape, dt=F32):
        return self.nc.dram_tensor(name, list(shape), dt, kind="Internal").ap()

    def layer_common(self, l):
        p = 'l%d_' % l
        return dict(ada_w=self.din(p + 'ada_w', [1024, 6144]), ada_b=self.din(p + 'ada_b', [6144]),
                    nm=self.din(p + 'norm_mix', [1024]), nf=self.din(p + 'norm_ffn', [1024]))

    def moe_in(self, l):
        p = 'l%d_' % l
        return dict(rw=self.din(p + 'router_w', [1024, 36]), rb=self.din(p + 'router_b', [36]),
                    ewi=self.din(p + 'expert_w_in', [32, 1024, 1024]), ewo=self.din(p + 'expert_w_out', [32, 512, 1024]))

    def moe_scratch(self, tag, NT):
        NB = 2 * NT + 32
        return {'hs': self.dint(tag + "hs", [NT * 128, 1024], BF16), 'xpad': self.dint(tag + "xpad", [NB * 128, 1024], BF16),
                'ys': self.dint(tag + "ys", [NB * 128, 1024], F32)}


def _consts_in(pg, NT, with_moe=True):
    cd = {n: pg.din("c_" + n, s) for n, s in CONST_SHAPES.items()}
    mc = {n: pg.din("mc_" + n, s) for n, s in moe_const_shapes(NT).items()} if with_moe else None
    return cd, mc


def build_L1(NT):
    pg = Prog(); cx = pg.cx
    x = pg.din("x", [NT * 128, 1024]); cT = pg.din("cT", [128, 8])
    cd, mc = _consts_in(pg, NT)
    lc = pg.layer_common(0); mi = pg.moe_in(0)
    gw = dict(w_in=pg.din("l0_gm_w_in", [1024, 2048]), v_norm=pg.din("l0_gm_v_norm", [1024]), w_s=pg.din("l0_gm_w_s", [8, 128, 128]),
              b_sT=pg.din("l0_gm_b_sT", [128, 8]), w_out=pg.din("l0_gm_w_out", [1024, 1024]))
    xa = pg.dint("xa", [NT * 128, 1024]); y = pg.dout("y", [NT * 128, 1024]); scr = pg.moe_scratch("m0", NT)
    with cx.stack:
        load_consts(cx, cd)
        mods = alloc_mods(cx)
        with cx.scope():
            emit_mod(cx, cT, lc['ada_w'], lc['ada_b'], lc['nm'], lc['nf'], mods)
        with cx.scope():
            emit_gmlp(cx, x, xa, NT, mods, gw['w_in'], gw['v_norm'], gw['w_s'], gw['b_sT'], gw['w_out'])
        with cx.scope():
            emit_moe(cx, xa, y, NT, mods, mi['rw'], mi['rb'], mi['ewi'], mi['ewo'], mc, scr)
        cx.S.emit()
    return pg.nc


def build_L2(NT):
    pg = Prog(); cx = pg.cx
    x = pg.din("x", [NT * 128, 1024]); cT = pg.din("cT", [128, 8]); xh = pg.din("xh", [128, 1024]); hp = pg.din("hasprev", [128, 1])
    cd, _ = _consts_in(pg, NT, with_moe=False)
    lc = pg.layer_common(1)
    w_in = pg.din("l1_dn_w_in", [1024, 3088]); cwT = pg.din("l1_dn_conv_wT", [2048, 4]); al = pg.din("l1_dn_a_log", [8]); dtb = pg.din("l1_dn_dt_bias", [8])
    oloc = pg.dout("oloc", [NT * 128, 1024]); RT = pg.dout("RT", [8, 128, NT * 128], BF16); zs = pg.dout("zs", [NT * 128, 1024], BF16); st = pg.dout("st", [8, 128, 256])
    with cx.stack:
        load_consts(cx, cd)
        mods = alloc_mods(cx)
        with cx.scope():
            emit_mod(cx, cT, lc['ada_w'], lc['ada_b'], lc['nm'], lc['nf'], mods)
        with cx.scope():
            emit_dn1(cx, x, NT, mods, w_in, cwT, al, dtb, xh, hp, oloc, RT, zs, st)
        cx.S.emit()
    return pg.nc


def build_L3(NT, nprev):
    pg = Prog(); cx = pg.cx
    x = pg.din("x", [NT * 128, 1024]); cT = pg.din("cT", [128, 8])
    cd, mc = _consts_in(pg, NT)
    lc = pg.layer_common(1); mi = pg.moe_in(1)
    oloc = pg.din("oloc", [NT * 128, 1024]); RT = pg.din("RT", [8, 128, NT * 128], BF16); zs = pg.din("zs", [NT * 128, 1024], BF16)
    pPT = pg.din("prevPT", [nprev, 8, 128, 128]); pS = pg.din("prevS", [nprev, 8, 128, 128])
    on = pg.din("l1_dn_o_norm", [128]); w_out = pg.din("l1_dn_w_out", [1024, 1024])
    xa = pg.dint("xa", [NT * 128, 1024]); y = pg.dout("y", [NT * 128, 1024]); scr = pg.moe_scratch("m1", NT)
    with cx.stack:
        load_consts(cx, cd)
        mods = alloc_mods(cx)
        with cx.scope():
            emit_mod(cx, cT, lc['ada_w'], lc['ada_b'], lc['nm'], lc['nf'], mods)
        with cx.scope():
            emit_dn2(cx, x, xa, NT, mods, pPT, pS, nprev, oloc, RT, zs, on, w_out)
        with cx.scope():
            emit_moe(cx, xa, y, NT, mods, mi['rw'], mi['rb'], mi['ewi'], mi['ewo'], mc, scr)
        cx.S.emit()
    return pg.nc


def build_L4(NT):
    pg = Prog(); cx = pg.cx
    x = pg.din("x", [NT * 128, 1024]); cT = pg.din("cT", [128, 8]); xh = pg.din("xh", [128, 1024]); mp0 = pg.din("mp0", [128, 128]); gtT = pg.din("gtT", [128, 128])
    cd, mc = _consts_in(pg, NT)
    lc2 = pg.layer_common(2); mi2 = pg.moe_in(2); lc3 = pg.layer_common(3); mi3 = pg.moe_in(3)
    sw = dict(w_in=pg.din("l2_swa_w_in", [1024, 1536]), qn=pg.din("l2_swa_q_norm", [64]), kn=pg.din("l2_swa_k_norm", [64]), sk=pg.din("l2_swa_sinks", [16]),
              w_out=pg.din("l2_swa_w_out", [1024, 1024]))
    gw = dict(w_in=pg.din("l3_gm_w_in", [1024, 2048]), v_norm=pg.din("l3_gm_v_norm", [1024]), w_s=pg.din("l3_gm_w_s", [8, 128, 128]),
              b_sT=pg.din("l3_gm_b_sT", [128, 8]), w_out=pg.din("l3_gm_w_out", [1024, 1024]))
    xa = pg.dint("xa", [NT * 128, 1024]); xb = pg.dint("xb", [NT * 128, 1024]); y = pg.dout("y", [NT * 128, 1024]); scr = pg.moe_scratch("m2", NT)
    with cx.stack:
        load_consts(cx, cd)
        mods = alloc_mods(cx)
        with cx.scope():
            emit_mod(cx, cT, lc2['ada_w'], lc2['ada_b'], lc2['nm'], lc2['nf'], mods)
        with cx.scope():
            emit_swa(cx, x, xa, NT, mods, sw['w_in'], sw['qn'], sw['kn'], sw['sk'], sw['w_out'], xh, mp0, gtT)
        with cx.scope():
            emit_moe(cx, xa, xb, NT, mods, mi2['rw'], mi2['rb'], mi2['ewi'], mi2['ewo'], mc, scr)
        with cx.scope():
            emit_mod(cx, cT, lc3['ada_w'], lc3['ada_b'], lc3['nm'], lc3['nf'], mods)
        with cx.scope():
            emit_gmlp(cx, xb, xa, NT, mods, gw['w_in'], gw['v_norm'], gw['w_s'], gw['b_sT'], gw['w_out'])
        with cx.scope():
            emit_moe(cx, xa, y, NT, mods, mi3['rw'], mi3['rb'], mi3['ewi'], mi3['ewo'], mc, scr)
        cx.S.emit()
    return pg.nc


def _f32(a):
    return np.ascontiguousarray(np.asarray(a, dtype=np.float32))


def kernel(**inputs):
    NT = NT_FULL
    inp = {k: _f32(v) for k, v in inputs.items()}
    x = inp['x']; c = inp['c']
    hc = host_consts(); mh = moe_host_consts(NT)
    ii = np.arange(128)
    gtT = (ii[:, None] > ii[None, :]).astype(np.float32)
    cores = list(range(NCORES))

    def seg(arr, core):
        b, q = core // 4, core % 4
        return np.ascontiguousarray(arr[b, q * TPC:(q + 1) * TPC])

    def halo(arr, core):
        b, q = core // 4, core % 4
        if q == 0:
            return np.zeros((128, 1024), np.float32)
        return np.ascontiguousarray(arr[b, q * TPC - 128:q * TPC])

    def base(core, with_moe=True):
        b = core // 4
        m = {"cT": np.ascontiguousarray(c[b].reshape(8, 128).T)}
        for n in CONST_SHAPES:
            m["c_" + n] = hc[n]
        if with_moe:
            for n in mh:
                m["mc_" + n] = mh[n]
        return m

    def layer_w(m, l, names):
        p = 'l%d_' % l
        for n in ['ada_w', 'ada_b', 'norm_mix', 'norm_ffn'] + names:
            m[p + n] = inp[p + n]

    def assemble(results, key="y"):
        out = np.zeros((2, 16384, 1024), np.float32)
        for core in cores:
            b, q = core // 4, core % 4
            out[b, q * TPC:(q + 1) * TPC] = results[core][key]
        return out

    moe_names = ['router_w', 'router_b', 'expert_w_in', 'expert_w_out']
    nc1 = build_L1(NT)
    maps = []
    for core in cores:
        m = base(core); m["x"] = seg(x, core)
        layer_w(m, 0, ['gm_w_in', 'gm_v_norm', 'gm_w_s', 'gm_w_out'] + moe_names)
        m['l0_gm_b_sT'] = np.ascontiguousarray(inp['l0_gm_b_s'].T)
        maps.append(m)
    r1 = run_bass_kernel_spmd(nc1, maps, core_ids=cores).results
    x1 = assemble(r1)
    nc2 = build_L2(NT)
    maps = []
    for core in cores:
        m = base(core, with_moe=False); m["x"] = seg(x1, core); m["xh"] = halo(x1, core)
        m["hasprev"] = np.full((128, 1), 1.0 if core % 4 > 0 else 0.0, np.float32)
        layer_w(m, 1, ['dn_w_in', 'dn_a_log', 'dn_dt_bias'])
        m['l1_dn_conv_wT'] = np.ascontiguousarray(inp['l1_dn_conv_w'].T)
        maps.append(m)
    r2 = run_bass_kernel_spmd(nc2, maps, core_ids=cores).results
    nprev = 3
    nc3 = build_L3(NT, nprev)
    maps = []
    for core in cores:
        b, q = core // 4, core % 4
        m = base(core); m["x"] = seg(x1, core)
        pPT = np.zeros((nprev, 8, 128, 128), np.float32); pS = np.zeros((nprev, 8, 128, 128), np.float32)
        for j in range(nprev):
            sq_ = q - nprev + j
            if sq_ >= 0:
                stt = np.asarray(r2[b * 4 + sq_]['st'])
                pS[j] = stt[:, :, 0:128]
                pPT[j] = stt[:, :, 128:256].transpose(0, 2, 1)
        m.update({"oloc": r2[core]['oloc'], "RT": r2[core]['RT'], "zs": r2[core]['zs'], "prevPT": pPT, "prevS": pS})
        layer_w(m, 1, ['dn_o_norm', 'dn_w_out'] + moe_names)
        maps.append(m)
    r3 = run_bass_kernel_spmd(nc3, maps, core_ids=cores).results
    x2 = assemble(r3)
    nc4 = build_L4(NT)
    maps = []
    for core in cores:
        m = base(core); m["x"] = seg(x2, core); m["xh"] = halo(x2, core)
        m["mp0"] = gtT if core % 4 > 0 else np.zeros((128, 128), np.float32)
        m["gtT"] = gtT
        layer_w(m, 2, ['swa_w_in', 'swa_q_norm', 'swa_k_norm', 'swa_sinks', 'swa_w_out'] + moe_names)
        layer_w(m, 3, ['gm_w_in', 'gm_v_norm', 'gm_w_s', 'gm_w_out'] + moe_names)
        m['l3_gm_b_sT'] = np.ascontiguousarray(inp['l3_gm_b_s'].T)
        maps.append(m)
    r4 = run_bass_kernel_spmd(nc4, maps, core_ids=cores).results
    return assemble(r4)
```
